# Optimizing a Trainium2 kernel written in Bass

```python
import math
import jax, jax.numpy as jnp
from jax import lax
import numpy as np

D_MODEL = 1024
BATCH = 16
SEQ = 2048
DEPTH = 2

HEAD_DIM = 64
MLA_HEADS = 6
MLA_Q_RANK = 384
MLA_KV_RANK = 256
MLA_NOPE = 64
MLA_ROPE = 32
MLA_V = 64
ROPE_THETA = 10000.0
FOX_HEADS = 6
FOX_FORGET_BIAS = 3.0
NSA_HEADS = 4
NSA_BRANCHES = 3
NSA_CMP_BLOCK = 32
NSA_CMP_STRIDE = 16
NSA_CMP_HIDDEN = 128
NSA_SEL_BLOCK = 64
NSA_SEL_TOPN = 8
NSA_WINDOW = 256
NSA_FORCE_SCORE = 1.0e4
Q_BLOCK = 128
D_FF = 2816
N_EXPERTS = 8
TOP_K = 2
D_FF_EXPERT = 1408
NORM_EPS = 1e-5
DEEPNORM_ALPHA = (2 * DEPTH) ** 0.25
DEEPNORM_BETA = (8 * DEPTH) ** -0.25
MLA_COLS = MLA_Q_RANK + MLA_KV_RANK + MLA_ROPE
FOX_COLS = 3 * FOX_HEADS * HEAD_DIM + FOX_HEADS
NSA_COLS = NSA_HEADS * HEAD_DIM + 2 * NSA_BRANCHES * HEAD_DIM + NSA_HEADS * NSA_BRANCHES
IN_COLS = MLA_COLS + FOX_COLS + NSA_COLS
MIX_WIDTH = MLA_HEADS * MLA_V + FOX_HEADS * HEAD_DIM + NSA_HEADS * HEAD_DIM
N_DENSE_LAYERS = (DEPTH + 1) // 2
N_MOE_LAYERS = DEPTH // 2

kernel_name = "hymba_mla_fox_nsa_deepnorm_moe"


def layer_norm(x, g, b):
    xf = x.astype(jnp.float32)
    mu = jnp.mean(xf, -1, keepdims=True)
    var = jnp.mean(jnp.square(xf - mu), -1, keepdims=True)
    return ((xf - mu) * lax.rsqrt(var + NORM_EPS) * g + b).astype(x.dtype)


def rms_norm(x, g):
    xf = x.astype(jnp.float32)
    return (xf * lax.rsqrt(jnp.mean(xf * xf, -1, keepdims=True) + NORM_EPS) * g).astype(x.dtype)


def masked_softmax(scores, mask):
    s = jnp.where(mask, scores, -1e30)
    m = jnp.max(s, -1, keepdims=True)
    p = jnp.where(mask, jnp.exp(s - m), 0.0)
    return p / jnp.maximum(jnp.sum(p, -1, keepdims=True), 1e-30)


def alibi_slopes(n):
    return jnp.asarray([2.0 ** (-8.0 * (i + 1) / n) for i in range(n)], jnp.float32)


def rope(x, pos):
    half = x.shape[-1] // 2
    freqs = ROPE_THETA ** (-jnp.arange(half, dtype=jnp.float32) / half)
    ang = pos.astype(jnp.float32)[:, None] * freqs[None, :]
    cos, sin = jnp.cos(ang), jnp.sin(ang)
    x1 = x[..., :half].astype(jnp.float32)
    x2 = x[..., half:].astype(jnp.float32)
    return jnp.concatenate([x1 * cos - x2 * sin, x2 * cos + x1 * sin], -1).astype(x.dtype)


def split_cols(z, sizes):
    offs = np.cumsum(np.asarray(sizes))[:-1].tolist()
    return jnp.split(z, offs, axis=-1)


def blocked_causal_attention(q, k, v, decay_cum=None):
    B, H, S, dk = q.shape
    nb = S // Q_BLOCK
    scale = dk ** -0.5
    k_pos = jnp.arange(S)
    q_blocks = jnp.moveaxis(q.reshape(B, H, nb, Q_BLOCK, dk), 2, 0)
    c_blocks = None if decay_cum is None else jnp.moveaxis(decay_cum.reshape(B, H, nb, Q_BLOCK), 2, 0)

    def one_block(args):
        i, q_blk, c_blk = args
        s = jnp.einsum("bhqd,bhkd->bhqk", q_blk, k).astype(jnp.float32) * scale
        if c_blk is not None:
            s = s + (c_blk[..., :, None] - decay_cum[..., None, :])
        q_pos = i * Q_BLOCK + jnp.arange(Q_BLOCK)
        p = masked_softmax(s, q_pos[:, None] >= k_pos[None, :])
        return jnp.einsum("bhqk,bhkd->bhqd", p.astype(v.dtype), v)

    o = lax.map(one_block, (jnp.arange(nb), q_blocks, c_blocks))
    return jnp.moveaxis(o, 0, 2).reshape(B, H, S, v.shape[-1])


def mla_mixer(cq_raw, ckv_raw, kr_raw, pos, g_cq, w_uq, g_ckv, w_ukv):
    B, S, _ = cq_raw.shape
    H = MLA_HEADS
    q = (rms_norm(cq_raw, g_cq) @ w_uq).reshape(B, S, H, MLA_NOPE + MLA_ROPE).transpose(0, 2, 1, 3)
    q = jnp.concatenate([q[..., :MLA_NOPE], rope(q[..., MLA_NOPE:], pos)], -1)
    kv = (rms_norm(ckv_raw, g_ckv) @ w_ukv).reshape(B, S, H, MLA_NOPE + MLA_V).transpose(0, 2, 1, 3)
    k_nope, v = kv[..., :MLA_NOPE], kv[..., MLA_NOPE:]
    k_rope = rope(kr_raw, pos)
    k = jnp.concatenate([k_nope, jnp.broadcast_to(k_rope[:, None], (B, H, S, MLA_ROPE))], -1)
    o = blocked_causal_attention(q, k, v)
    return o.transpose(0, 2, 1, 3).reshape(B, S, H * MLA_V)


def fox_mixer(qkv, f_logit, b_forget):
    B, S, _ = qkv.shape
    H = FOX_HEADS
    qkv = qkv.reshape(B, S, 3, H, HEAD_DIM).transpose(2, 0, 3, 1, 4)
    log_f = jax.nn.log_sigmoid(f_logit.astype(jnp.float32) + b_forget)
    c = jnp.cumsum(log_f, axis=1).transpose(0, 2, 1)
    o = blocked_causal_attention(qkv[0], qkv[1], qkv[2], c)
    return o.transpose(0, 2, 1, 3).reshape(B, S, H * HEAD_DIM)


def nsa_mixer(q, k_cmp_raw, v_cmp_raw, k_slc, v_slc, k_win, v_win, gate_logit,
              cmp_k_pos, cmp_k_w1, cmp_k_w2, cmp_v_pos, cmp_v_w1, cmp_v_w2):
    B, S, _ = q.shape
    H = NSA_HEADS
    f32 = jnp.float32
    q = q.reshape(B, S, H, HEAD_DIM).transpose(0, 2, 1, 3)
    scale = HEAD_DIM ** -0.5
    slopes = alibi_slopes(H)
    pos = jnp.arange(S)

    n_cmp = (S - NSA_CMP_BLOCK) // NSA_CMP_STRIDE + 1
    starts = jnp.arange(n_cmp) * NSA_CMP_STRIDE
    blk_idx = starts[:, None] + jnp.arange(NSA_CMP_BLOCK)[None, :]

    def compress(t, pos_emb, w1, w2):
        blocks = t[:, blk_idx] + pos_emb
        return jax.nn.silu(blocks.reshape(B, n_cmp, NSA_CMP_BLOCK * HEAD_DIM) @ w1) @ w2

    k_c = compress(k_cmp_raw, cmp_k_pos, cmp_k_w1, cmp_k_w2)
    v_c = compress(v_cmp_raw, cmp_v_pos, cmp_v_w1, cmp_v_w2)
    blk_end = starts + NSA_CMP_BLOCK - 1
    dist_c = (pos[:, None] - blk_end[None, :]).astype(f32)
    s_c = jnp.einsum("bhtd,bnd->bhtn", q, k_c).astype(f32) * scale - slopes[:, None, None] * dist_c
    p_c = masked_softmax(s_c, dist_c >= 0)
    o_c = jnp.einsum("bhtn,bnd->bhtd", p_c.astype(v_c.dtype), v_c)

    n_blk = S // NSA_SEL_BLOCK
    n_sel = min(NSA_SEL_TOPN, n_blk)
    sel_start = jnp.arange(n_blk) * NSA_SEL_BLOCK
    overlap = ((starts[:, None] < sel_start[None, :] + NSA_SEL_BLOCK)
               & (starts[:, None] + NSA_CMP_BLOCK > sel_start[None, :])).astype(f32)
    imp = jnp.einsum("bhtn,nj->btj", p_c, overlap)
    cur = pos // NSA_SEL_BLOCK
    blk_ids = jnp.arange(n_blk)
    forced = (blk_ids[None, :] == 0) | (blk_ids[None, :] == cur[:, None]) | (blk_ids[None, :] == cur[:, None] - 1)
    future = sel_start[None, :] > pos[:, None]
    imp = jnp.where(forced, NSA_FORCE_SCORE, jnp.where(future, -1.0, imp))

    nb = S // Q_BLOCK
    q_blocks = jnp.moveaxis(q.reshape(B, H, nb, Q_BLOCK, HEAD_DIM), 2, 0)
    imp_blocks = jnp.moveaxis(imp.reshape(B, nb, Q_BLOCK, n_blk), 1, 0)
    pad = jnp.zeros((B, NSA_WINDOW, HEAD_DIM), k_win.dtype)
    k_win_pad = jnp.concatenate([pad, k_win], 1)
    v_win_pad = jnp.concatenate([pad, v_win], 1)
    win_len = Q_BLOCK + NSA_WINDOW

    def sel_win_block(args):
        i, q_blk, imp_blk = args
        q_pos = i * Q_BLOCK + jnp.arange(Q_BLOCK)
        _, top = lax.top_k(imp_blk, n_sel)
        tok = (top[..., None] * NSA_SEL_BLOCK + jnp.arange(NSA_SEL_BLOCK)).reshape(B, Q_BLOCK, n_sel * NSA_SEL_BLOCK)
        k_g = jax.vmap(lambda kk, ii: kk[ii])(k_slc, tok)
        v_g = jax.vmap(lambda vv, ii: vv[ii])(v_slc, tok)
        dist_s = (q_pos[None, :, None] - tok).astype(f32)
        s_s = jnp.einsum("bhqd,bqkd->bhqk", q_blk, k_g).astype(f32) * scale - slopes[None, :, None, None] * dist_s[:, None]
        p_s = masked_softmax(s_s, (dist_s >= 0)[:, None])
        o_s = jnp.einsum("bhqk,bqkd->bhqd", p_s.astype(v_g.dtype), v_g)
        k_w = lax.dynamic_slice_in_dim(k_win_pad, i * Q_BLOCK, win_len, axis=1)
        v_w = lax.dynamic_slice_in_dim(v_win_pad, i * Q_BLOCK, win_len, axis=1)
        kp = i * Q_BLOCK - NSA_WINDOW + jnp.arange(win_len)
        d_w = q_pos[:, None] - kp[None, :]
        m_w = (d_w >= 0) & (d_w < NSA_WINDOW) & (kp >= 0)[None, :]
        s_w = jnp.einsum("bhqd,bkd->bhqk", q_blk, k_w).astype(f32) * scale - slopes[:, None, None] * d_w.astype(f32)
        p_w = masked_softmax(s_w, m_w)
        o_w = jnp.einsum("bhqk,bkd->bhqd", p_w.astype(v_w.dtype), v_w)
        return o_s, o_w

    o_s, o_w = lax.map(sel_win_block, (jnp.arange(nb), q_blocks, imp_blocks))
    o_s = jnp.moveaxis(o_s, 0, 2).reshape(B, H, S, HEAD_DIM)
    o_w = jnp.moveaxis(o_w, 0, 2).reshape(B, H, S, HEAD_DIM)

    g = jax.nn.sigmoid(gate_logit.astype(f32)).reshape(B, S, H, NSA_BRANCHES).transpose(0, 2, 1, 3)
    o = (g[..., 0:1] * o_c.astype(f32) + g[..., 1:2] * o_s.astype(f32) + g[..., 2:3] * o_w.astype(f32)).astype(q.dtype)
    return o.transpose(0, 2, 1, 3).reshape(B, S, H * HEAD_DIM)


def hybrid_mixer(x, pos, w_in, b_forget, g_cq, w_uq, g_ckv, w_ukv,
                 cmp_k_pos, cmp_k_w1, cmp_k_w2, cmp_v_pos, cmp_v_w1, cmp_v_w2, w_out):
    z = x @ w_in
    (cq, ckv, kr, fox_qkv, fox_f, nsa_q, k_cmp, v_cmp, k_slc, v_slc, k_win, v_win, nsa_g) = split_cols(
        z, [MLA_Q_RANK, MLA_KV_RANK, MLA_ROPE,
            3 * FOX_HEADS * HEAD_DIM, FOX_HEADS,
            NSA_HEADS * HEAD_DIM, HEAD_DIM, HEAD_DIM, HEAD_DIM, HEAD_DIM, HEAD_DIM, HEAD_DIM,
            NSA_HEADS * NSA_BRANCHES])
    o_mla = mla_mixer(cq, ckv, kr, pos, g_cq, w_uq, g_ckv, w_ukv)
    o_fox = fox_mixer(fox_qkv, fox_f, b_forget)
    o_nsa = nsa_mixer(nsa_q, k_cmp, v_cmp, k_slc, v_slc, k_win, v_win, nsa_g,
                      cmp_k_pos, cmp_k_w1, cmp_k_w2, cmp_v_pos, cmp_v_w1, cmp_v_w2)
    return jnp.concatenate([o_mla, o_fox, o_nsa], -1) @ w_out


def swiglu(h, w1, w3, w2):
    return (jax.nn.silu(h @ w1) * (h @ w3)) @ w2


def moe_swiglu(x, router_w, w1, w3, w2):
    B, S, D = x.shape
    h = x.reshape(B * S, D)
    probs = jax.nn.softmax((h @ router_w).astype(jnp.float32), -1)
    top_p, top_i = lax.top_k(probs, TOP_K)
    top_p = top_p / jnp.sum(top_p, -1, keepdims=True)
    gates = jnp.sum(jax.nn.one_hot(top_i, N_EXPERTS, dtype=jnp.float32) * top_p[..., None], axis=1)
    out = jnp.zeros_like(h)
    for e in range(N_EXPERTS):
        out = out + gates[:, e:e + 1].astype(h.dtype) * swiglu(h, w1[e], w3[e], w2[e])
    return out.reshape(B, S, D)


def setup_inputs(seed: int = 0) -> dict:
    key = jax.random.key(seed)
    ks = iter(jax.random.split(key, 32))
    L = DEPTH
    nrm = lambda shape, scale: jax.random.normal(next(ks), shape, jnp.float32) * scale
    return {
        "x": nrm((BATCH, SEQ, D_MODEL), 1.0),
        "w_in": nrm((L, D_MODEL, IN_COLS), D_MODEL ** -0.5),
        "b_forget": FOX_FORGET_BIAS + nrm((L, FOX_HEADS), 0.1),
        "g_cq": 1.0 + nrm((L, MLA_Q_RANK), 0.02),
        "w_uq": nrm((L, MLA_Q_RANK, MLA_HEADS * (MLA_NOPE + MLA_ROPE)), MLA_Q_RANK ** -0.5),
        "g_ckv": 1.0 + nrm((L, MLA_KV_RANK), 0.02),
        "w_ukv": nrm((L, MLA_KV_RANK, MLA_HEADS * (MLA_NOPE + MLA_V)), MLA_KV_RANK ** -0.5),
        "cmp_k_pos": nrm((L, NSA_CMP_BLOCK, HEAD_DIM), 0.02),
        "cmp_k_w1": nrm((L, NSA_CMP_BLOCK * HEAD_DIM, NSA_CMP_HIDDEN), (NSA_CMP_BLOCK * HEAD_DIM) ** -0.5),
        "cmp_k_w2": nrm((L, NSA_CMP_HIDDEN, HEAD_DIM), NSA_CMP_HIDDEN ** -0.5),
        "cmp_v_pos": nrm((L, NSA_CMP_BLOCK, HEAD_DIM), 0.02),
        "cmp_v_w1": nrm((L, NSA_CMP_BLOCK * HEAD_DIM, NSA_CMP_HIDDEN), (NSA_CMP_BLOCK * HEAD_DIM) ** -0.5),
        "cmp_v_w2": nrm((L, NSA_CMP_HIDDEN, HEAD_DIM), NSA_CMP_HIDDEN ** -0.5),
        "w_out": nrm((L, MIX_WIDTH, D_MODEL), MIX_WIDTH ** -0.5 * DEEPNORM_BETA),
        "ln1_g": 1.0 + nrm((L, D_MODEL), 0.02),
        "ln1_b": nrm((L, D_MODEL), 0.02),
        "ln2_g": 1.0 + nrm((L, D_MODEL), 0.02),
        "ln2_b": nrm((L, D_MODEL), 0.02),
        "ffn_w1": nrm((N_DENSE_LAYERS, D_MODEL, D_FF), D_MODEL ** -0.5),
        "ffn_w3": nrm((N_DENSE_LAYERS, D_MODEL, D_FF), D_MODEL ** -0.5),
        "ffn_w2": nrm((N_DENSE_LAYERS, D_FF, D_MODEL), D_FF ** -0.5 * DEEPNORM_BETA),
        "router_w": nrm((N_MOE_LAYERS, D_MODEL, N_EXPERTS), D_MODEL ** -0.5),
        "moe_w1": nrm((N_MOE_LAYERS, N_EXPERTS, D_MODEL, D_FF_EXPERT), D_MODEL ** -0.5),
        "moe_w3": nrm((N_MOE_LAYERS, N_EXPERTS, D_MODEL, D_FF_EXPERT), D_MODEL ** -0.5),
        "moe_w2": nrm((N_MOE_LAYERS, N_EXPERTS, D_FF_EXPERT, D_MODEL), D_FF_EXPERT ** -0.5 * DEEPNORM_BETA),
    }


def reference(x, w_in, b_forget, g_cq, w_uq, g_ckv, w_ukv,
              cmp_k_pos, cmp_k_w1, cmp_k_w2, cmp_v_pos, cmp_v_w1, cmp_v_w2,
              w_out, ln1_g, ln1_b, ln2_g, ln2_b,
              ffn_w1, ffn_w3, ffn_w2, router_w, moe_w1, moe_w3, moe_w2):
    pos = jnp.arange(x.shape[1])
    for layer in range(DEPTH):
        mix = hybrid_mixer(x, pos, w_in[layer], b_forget[layer], g_cq[layer], w_uq[layer], g_ckv[layer], w_ukv[layer],
                           cmp_k_pos[layer], cmp_k_w1[layer], cmp_k_w2[layer],
                           cmp_v_pos[layer], cmp_v_w1[layer], cmp_v_w2[layer], w_out[layer])
        x = layer_norm(DEEPNORM_ALPHA * x + mix, ln1_g[layer], ln1_b[layer])
        j = layer // 2
        if layer % 2 == 0:
            f = swiglu(x, ffn_w1[j], ffn_w3[j], ffn_w2[j])
        else:
            f = moe_swiglu(x, router_w[j], moe_w1[j], moe_w3[j], moe_w2[j])
        x = layer_norm(DEEPNORM_ALPHA * x + f, ln2_g[layer], ln2_b[layer])
    return x
```

```python
import numpy as np
import ml_dtypes
from contextlib import ExitStack
import concourse.bass as bass
import concourse.mybir as mybir
from concourse.bass_utils import run_bass_kernel_spmd

F32 = mybir.dt.float32
BF16 = mybir.dt.bfloat16
AF = mybir.ActivationFunctionType
ALU = mybir.AluOpType

S = 2048
D = 1024
NT = 16
NSPAN = 4
DEPTH = 2
ALPHA = (2 * DEPTH) ** 0.25
EPS = 1e-5
NEG = -30720.0
C_CQ, C_CKV, C_KR = 0, 384, 640
C_FQ, C_FK, C_FV, C_FF = 672, 1056, 1440, 1824
C_NQ, C_KCMP, C_VCMP, C_KSLC, C_VSLC, C_KWIN, C_VWIN, C_GATE = 1830, 2086, 2150, 2214, 2278, 2342, 2406, 2470
IN_COLS = 2482
D_FF = 2816
N_EXP = 8
D_FFE = 1408

ENGS = ("pe", "act", "dve", "pool", "sp")


class Op:
    __slots__ = ("eng", "fn", "waits", "signal", "count", "dma_sem", "is_dma")

    def __init__(self, eng, fn):
        self.eng = eng
        self.fn = fn
        self.waits = []
        self.signal = False
        self.count = 0
        self.dma_sem = None
        self.is_dma = False


class Prog:
    def __init__(self, nc):
        self.nc = nc
        self.streams = {e: [] for e in ENGS}
        self.bufs = {}
        self.dma_cnt = {}
        self.fence_op = None

    def _deps(self, rd, wr):
        deps = []
        if self.fence_op is not None:
            for k in list(rd) + list(wr):
                if k not in self.bufs:
                    deps.append(self.fence_op)
                    break
        for k in rd:
            b = self.bufs.get(k)
            if b and b[0] is not None:
                deps.append(b[0])
        for k in wr:
            b = self.bufs.get(k)
            if b:
                if b[0] is not None:
                    deps.append(b[0])
                deps.extend(b[1])
        return deps

    def _register(self, op, rd, wr):
        for k in rd:
            b = self.bufs.setdefault(k, [None, []])
            b[1].append(op)
        for k in wr:
            self.bufs[k] = [op, []]

    def op(self, eng, fn, rd=(), wr=()):
        o = Op(eng, fn)
        for d in self._deps(rd, wr):
            if d.is_dma:
                o.waits.append(("dma", d.dma_sem, self.dma_cnt[d.dma_sem]))
            else:
                if d.eng == eng and eng == "pe":
                    continue
                d.signal = True
                o.waits.append(("op", d, 0))
        self._register(o, rd, wr)
        self.streams[eng].append(o)
        return o

    def fence(self, fn):
        o = self.op("dve", fn, rd=(), wr=list(self.bufs.keys()))
        self.bufs = {}
        self.fence_op = o
        return o

    def dma(self, eng, out, in_, sem, rd=(), wr=(), newgroup=True, **kw):
        o = self.op(eng, lambda e: e.dma_start(out=out, in_=in_, **kw), rd, wr)
        o.is_dma = True
        o.dma_sem = sem
        prev = self.dma_cnt.get(sem, 0)
        if newgroup and prev > 0:
            o.waits.append(("dma", sem, prev))
        self.dma_cnt[sem] = prev + 1
        return o

    def emit(self, es, final_waits):
        nc = self.nc
        sems = {e: es.enter_context(nc.semaphore("s_" + e)) for e in ENGS}
        dsems = {k: es.enter_context(nc.semaphore("d_" + k)) for k in self.dma_cnt}
        for e in ENGS:
            c = 0
            for o in self.streams[e]:
                if o.signal and not o.is_dma:
                    c += 1
                    o.count = c
        block = es.enter_context(nc.Block())
        streams = self.streams

        def run(e, eng):
            waited = {}
            for o in streams[e]:
                for w in o.waits:
                    if w[0] == "op":
                        key = ("e", w[1].eng)
                        val = w[1].count
                        sem = sems[w[1].eng]
                    else:
                        key = ("d", w[1])
                        val = 16 * w[2]
                        sem = dsems[w[1]]
                    if waited.get(key, 0) < val:
                        eng.wait_ge(sem, val)
                        waited[key] = val
                inst = o.fn(eng)
                if o.is_dma:
                    inst.then_inc(dsems[o.dma_sem], 16)
                elif o.signal:
                    inst.then_inc(sems[e], 1)
            if e == "sp":
                for semname in final_waits:
                    eng.wait_ge(dsems[semname], 16 * self.dma_cnt[semname])

        @block.tensor
        def _(t):
            run("pe", t)

        @block.scalar
        def _(t):
            run("act", t)

        @block.vector
        def _(t):
            run("dve", t)

        @block.gpsimd
        def _(t):
            run("pool", t)

        @block.sync
        def _(t):
            run("sp", t)


def host_consts():
    bf = ml_dtypes.bfloat16
    c = {}
    p = np.arange(128)
    maskC = np.where(p[:, None] <= p[None, :], 0.0, NEG).astype(np.float32)
    maskA = np.where(p[:, None] > p[None, :], 0.0, NEG).astype(np.float32)
    c["c_masks"] = np.stack([maskC, maskA], 1).astype(bf)
    t = np.arange(S)
    n = np.arange(128)
    mc = np.where(t[None, :] >= (16 * n[:, None] + 31), 0.0, NEG).astype(np.float32)
    c["c_maskcmp"] = mc.astype(bf)
    half = 16
    freqs = 10000.0 ** (-np.arange(half, dtype=np.float32) / half)
    ang = t.astype(np.float32)[None, :] * freqs[:, None]
    cos = np.cos(ang).astype(np.float32)
    sin = np.sin(ang).astype(np.float32)
    rope = np.zeros((2, 128, S), np.float32)
    rope[0, 64:80] = cos
    rope[0, 80:96] = cos
    rope[1, 64:80] = -sin
    rope[1, 80:96] = sin
    c["c_rope"] = rope.astype(bf)
    slopes = [2.0 ** (-8.0 * (i + 1) / 4) for i in range(4)]
    thi = (t // 16) * 16
    tlo = t % 16
    aq = np.zeros((4, 4, S), np.float32)
    for h in range(4):
        aq[h, 0] = -8 * slopes[h] * thi
        aq[h, 1] = -8 * slopes[h] * tlo
        aq[h, 2] = 8 * slopes[h]
        aq[h, 3] = 8 * slopes[h]
    c["c_alibi_q"] = aq.astype(bf)
    ak = np.zeros((2, 4, S), np.float32)
    ak[0, 0] = 1
    ak[0, 1] = 1
    ak[0, 2] = thi
    ak[0, 3] = tlo
    pc = 16 * np.arange(S) + 31
    ak[1, 0] = 1
    ak[1, 1] = 1
    ak[1, 2] = (pc // 16) * 16
    ak[1, 3] = pc % 16
    c["c_alibi_k"] = ak.astype(bf)
    E = (t[None, :] // 64 == np.arange(32)[:, None]).astype(np.float32)
    c["c_onehot"] = E.astype(bf)
    keep = np.ones((128, 16, 32), np.float32)
    add = np.zeros((128, 16, 32), np.float32)
    for tt in range(16):
        for pp in range(128):
            tok = tt * 128 + pp
            cur = tok // 64
            for j in range(32):
                forced = (j == 0) or (j == cur) or (j == cur - 1)
                future = j * 64 > tok
                if forced:
                    keep[pp, tt, j] = 0
                    add[pp, tt, j] = 1.0e4
                elif future:
                    keep[pp, tt, j] = 0
                    add[pp, tt, j] = -1.0
    c["c_keepadd"] = np.stack([keep, add], 1).astype(np.float32)
    starts = np.arange(127) * 16
    sel_start = np.arange(32) * 64
    ov = ((starts[:, None] < sel_start[None, :] + 64) & (starts[:, None] + 32 > sel_start[None, :])).astype(np.float32)
    ova = np.zeros((128, 33), np.float32)
    ova[:127, :32] = ov
    ova[:127, 32] = 1.0
    c["c_ovl"] = ova.astype(bf)
    selm = np.zeros((44, 12, 128), np.float32)
    for r in range(12):
        selm[r, r, :] = 1
        selm[32 + r, r, :] = 1
    c["c_gsel"] = selm.astype(bf)
    c["c_ones"] = np.ones((3, S), np.float32).astype(bf)
    c["c_negones"] = (-np.ones((3, S), np.float32)).astype(bf)
    return c


class Ctx:
    pass


def build(nseq=2, dbg=None, nlayers=DEPTH):
    dbg = dbg or {}
    nc = bass.Bass("TRN2", target_bir_lowering=False)
    P = Prog(nc)
    es = ExitStack()

    def din(name, shape, dt=F32):
        return nc.dram_tensor(name, list(shape), dt, kind="ExternalInput").ap()

    x_d = din("x", [nseq, S, D])
    w_in = din("w_in", [DEPTH, D, IN_COLS])
    w_krsw = din("w_krsw", [DEPTH, D, 96])
    b_forget = din("b_forget", [DEPTH, 6, 1])
    g_cq = din("g_cq", [DEPTH, 3, 128])
    w_uq = din("w_uq", [DEPTH, 384, 576])
    w_uq_sw = din("w_uq_sw", [DEPTH, 384, 576])
    g_ckv = din("g_ckv", [DEPTH, 2, 128])
    w_ukv = din("w_ukv", [DEPTH, 256, 768])
    cmp_posT = din("cmp_posT", [DEPTH, 2, 64, 32])
    cmp_w1 = din("cmp_w1", [DEPTH, 2, 2048, 128])
    cmp_w2 = din("cmp_w2", [DEPTH, 2, 128, 64])
    w_out = din("w_out", [DEPTH, D, D])
    ln_gb = din("ln_gb", [DEPTH, 4, D])
    ffn_w1 = din("ffn_w1", [1, D, D_FF])
    ffn_w3 = din("ffn_w3", [1, D, D_FF])
    ffn_w2 = din("ffn_w2", [1, D_FF, D])
    router_w = din("router_w", [1, D, N_EXP])
    moe_w1 = din("moe_w1", [1, N_EXP, D, D_FFE])
    moe_w3 = din("moe_w3", [1, N_EXP, D, D_FFE])
    moe_w2 = din("moe_w2", [1, N_EXP, D_FFE, D])
    hc = host_consts()
    cd = {k: din(k, v.shape, BF16 if v.dtype == ml_dtypes.bfloat16 else F32) for k, v in hc.items()}
    out_d = nc.dram_tensor("out", [nseq, S, D], F32, kind="ExternalOutput").ap()
    dbg_d = {k: nc.dram_tensor(k, list(shp), dt, kind="ExternalOutput").ap() for k, (shp, dt) in dbg.items()}

    def sb(name, shape, dt):
        return es.enter_context(nc.sbuf_tensor(name, list(shape), dt))

    xres = sb("xres", [128, NT, D], F32)
    xT = sb("xT", [128, 8, S], BF16)
    mixT = sb("mixT", [128, 8, S], BF16)
    wpool_t = sb("wpool", [128, 8 * 1024], BF16)
    wpool = [wpool_t[:, i * 1024:(i + 1) * 1024] for i in range(8)]
    QT = sb("QT", [128, S], BF16)
    KT = sb("KT", [128, S], BF16)
    QTs = [QT, wpool_t[:, 4096:6144]]
    KTs = [KT, wpool_t[:, 6144:8192]]
    wmod = [4]
    VA = [sb("VA0", [128, NT, 128], BF16), sb("VA1", [128, NT, 128], BF16)]
    PT = [sb("PT%d" % i, [128, 512], BF16) for i in range(3)]
    ident = sb("ident", [128, 128], BF16)
    masks = sb("masks", [128, 2, 128], BF16)
    ones_bf = sb("ones_bf", [128, 128], BF16)
    smalls = sb("smalls", [128, 64], F32)
    rs = sb("rs", [128, 512], F32)
    xb_ld = sb("xb_ld", [128, 1024], BF16)
    ARENA_B = 39 * 1024
    arena = sb("arena", [128, ARENA_B // 4], F32)
    psb = [es.enter_context(nc.psum_tensor("ps%d" % i, [128, 512], F32)) for i in range(8)]

    class Carver:
        def __init__(self):
            self.off = 0

        def take(self, nbytes, dt, shape=None):
            n4 = (nbytes + 3) // 4
            n4 = (n4 + 7) // 8 * 8
            assert (self.off + n4) * 4 <= ARENA_B, (self.off * 4, nbytes)
            v = arena[:, self.off:self.off + n4]
            self.off += n4
            if dt == BF16:
                v = v.bitcast(BF16)[:, 0:nbytes // 2]
            else:
                v = v[:, 0:nbytes // 4]
            return v

    wp_i = [0]

    wgen = {}

    def wslot():
        i = wp_i[0] % wmod[0]
        wp_i[0] += 1
        wgen[i] = True
        return ("wp", i), wpool[i]

    dmasem_i = [0]

    def wload(dst_ap, src_ap, slotkey, **kw):
        sem = "w%d" % slotkey[1]
        ng = wgen.get(slotkey[1], True)
        wgen[slotkey[1]] = False
        P.dma("pool", dst_ap, src_ap, sem, wr=[slotkey], newgroup=ng, **kw)

    gb_i = [0]

    def gbank():
        i = 6 + gb_i[0] % 2
        gb_i[0] += 1
        return ("ps", i), psb[i]

    sbk_i = [0]

    def sbank():
        i = sbk_i[0] % 4
        sbk_i[0] += 1
        return ("ps", i), psb[i]

    ob_i = [0]

    def obank():
        i = 4 + ob_i[0] % 2
        ob_i[0] += 1
        return ("ps", i), psb[i]

    pt_i = [0]

    def ptile():
        i = pt_i[0] % 3
        pt_i[0] += 1
        return ("PT", i), PT[i]

    def mm(out, lhsT, rhs, start, stop, rd, wr):
        P.op("pe", lambda e: e.matmul(out, lhsT=lhsT, rhs=rhs, start=start, stop=stop, skip_group_check=True),
             rd=rd, wr=wr)

    def act(out, in_, func, rd, wr, **kw):
        P.op("act", lambda e: e.activation(out=out, in_=in_, func=func, **kw), rd=rd, wr=wr)

    def dve(name, rd, wr, *a, **kw):
        P.op("dve", lambda e: getattr(e, name)(*a, **kw), rd=rd, wr=wr)

    def span_keys(name, sp):
        return [(name, 4 * sp + i) for i in range(4)]

    def all_keys(name):
        return [(name, i) for i in range(NT)]

    def w_in_view(l, c0, n):
        return w_in[l][:, c0:c0 + n].rearrange("(kc p) n -> p kc n", p=128)

    def load_win(l, c0, n, src=None):
        key, slot = wslot()
        v = slot[:, 0:8 * n].rearrange("p (a b) -> p a b", a=8)
        wload(v, src if src is not None else w_in_view(l, c0, n), key)
        return key, v

    def proj_fm(wkey, wv, m, sp, bank_key, bank, col0=0):
        for kc in range(8):
            mm(bank[0:m, :], wv[:, kc, col0:col0 + m], xT[:, kc, sp * 512:(sp + 1) * 512], kc == 0, kc == 7,
               rd=[wkey] + span_keys("xT", sp), wr=[bank_key])

    P.op("pool", lambda e: e.memset(ident[:], 0.0), wr=["ident"])
    P.op("pool", lambda e: e.affine_select(out=ident[:], in_=ident[:], pattern=[[-1, 128]], compare_op=ALU.not_equal,
                                           fill=1.0, base=0, channel_multiplier=1), rd=["ident"], wr=["ident"])
    P.op("pool", lambda e: e.memset(ones_bf[:], 1.0), wr=["ones_bf"])
    P.op("pool", lambda e: e.memset(smalls[:, 0:1], EPS), wr=["smalls_c"])
    P.op("pool", lambda e: e.memset(smalls[:, 1:2], 1.0), wr=["smalls_c"])
    P.op("pool", lambda e: e.memset(smalls[:, 4:5], 1e-30), wr=["smalls_c"])
    P.op("pool", lambda e: e.memset(VA[0][:], 1.0), wr=[("VA", 0)])
    P.op("pool", lambda e: e.memset(VA[1][:], 1.0), wr=[("VA", 1)])
    P.dma("sp", masks[:], cd["c_masks"], "cst", wr=["masks"])
    maskC = masks[:, 0, :]
    maskA = masks[:, 1, :]

    def transpose_to_xT(tt, src_key, xb, dstT, dst_name):
        bk, bank = gbank()
        bv = bank[:].bitcast(BF16)
        for c in range(8):
            P.op("pe", lambda e, c=c: e.transpose(bv[:, c * 128:(c + 1) * 128], xb[:, c * 128:(c + 1) * 128], ident[:]),
                 rd=[src_key, "ident"], wr=[bk])
        act(dstT[:, :, tt * 128:(tt + 1) * 128], bv.rearrange("p (c t) -> p c t", c=8), AF.Copy,
            rd=[], wr=[bk, (dst_name, tt)])

    def make_group(qt_key, kt_key, ktile, va_key, va, r0, r1, scale, sp, mode, post, qtile=None):
        QTt = QT if qtile is None else qtile
        q0 = sp * 512
        kts = list(range(0, 4 * sp + 4)) if mode == "causal" else list(range(max(0, 4 * sp - 2), 4 * sp + 4))
        tasks = []
        for kt in kts:
            lo = max(q0, kt * 128)
            hi = q0 + 512
            if mode == "window":
                hi = min(hi, (kt + 3) * 128)
            mk = []
            if kt * 128 >= q0:
                mk.append((kt * 128 - q0, 128, ident[:], maskC, ["ident", "masks"]))
            if mode == "window" and q0 <= (kt + 2) * 128 < q0 + 512:
                mk.append(((kt + 2) * 128 - q0, 128, ident[:], maskA, ["ident", "masks"]))
            tasks.append(dict(lhsT=ktile[r0:r1, kt * 128:(kt + 1) * 128], rhs=QTt[r0:r1, lo:hi], c0=lo - q0, c1=hi - q0,
                              masks=mk, prow=128, va=va[:, kt, :], rdk=[kt_key, qt_key], vak=va_key, scale=scale))
        return dict(tasks=tasks, post=post)

    def run_groups(groups, look=3, fillers=()):
        flat = [(g, i) for g in groups for i in range(len(g["tasks"]))]
        staged = {}
        fillers = list(fillers)
        fpos = [0]
        ntask = max(1, len(flat) - 4)

        def fill(idx):
            want = min(len(fillers), ((idx + 1) * len(fillers) + ntask - 1) // ntask)
            while fpos[0] < want:
                fillers[fpos[0]]()
                fpos[0] += 1

        def qk(idx):
            g, i = flat[idx]
            t = g["tasks"][i]
            skey, sbk = sbank()
            pr = t["prow"]
            mm(sbk[0:pr, t["c0"]:t["c1"]], t["lhsT"], t["rhs"], True, False, rd=t["rdk"], wr=[skey])
            for (off, w, idap, mkap, rdm) in t["masks"]:
                mm(sbk[0:pr, off:off + w], idap, mkap, False, False, rd=rdm, wr=[skey])
            staged[idx] = (skey, sbk)

        gorder = {id(g): k for k, g in enumerate(groups)}
        pending = []

        def flush(pred):
            keep = []
            for item in pending:
                if pred(item):
                    item[2]()
                else:
                    keep.append(item)
            pending[:] = keep

        for j in range(min(look, len(flat))):
            qk(j)
        for idx in range(len(flat)):
            if idx + look < len(flat):
                qk(idx + look)
            g, i = flat[idx]
            t = g["tasks"][i]
            n = len(g["tasks"])
            go = gorder[id(g)]
            if i == 0:
                flush(lambda it: it[1] <= go - 2)
                g["okey"], g["ob"] = obank()
            skey, sbk = staged.pop(idx)
            pkey, pt = ptile()
            pr = t["prow"]
            c0, c1 = t["c0"], t["c1"]
            act(pt[0:pr, c0:c1], sbk[0:pr, c0:c1], AF.Exp, rd=[], wr=[skey, pkey], scale=t["scale"])
            mm(g["ob"][:, c0:c1], t["va"], pt[0:pr, c0:c1], i == 0, i == n - 1, rd=[t["vak"], pkey], wr=[g["okey"]])
            if i == n - 1:
                pending.append((idx + 2, go, (lambda g=g: g["post"](g["okey"], g["ob"]))))
            flush(lambda it: it[0] <= idx)
            fill(idx)
        flush(lambda it: True)
        fill(10 ** 9)

    def post_simple(okey, ob, par, c, sp):
        a, b = (0, 64) if par == 0 else (64, 0)
        act(rs[a:a + 64, :], ob[b:b + 64, :], AF.Ln, rd=[], wr=[okey, "rs"])
        act(rs[a:a + 64, :], rs[a:a + 64, :], AF.Exp, rd=[], wr=["rs"], scale=-1.0)
        dve("tensor_tensor", ["rs"], [okey] + span_keys(("mixT", c), sp),
            out=mixT[a:a + 64, c, sp * 512:(sp + 1) * 512], in0=ob[a:a + 64, :], in1=rs[a:a + 64, :], op=ALU.mult)

    def dbg_dump(name, src_ap, rd):
        if name in dbg_d:
            P.dma("sp", dbg_d[name], src_ap, "dbg", rd=rd)

    def mla_phase(l):
        cv = Carver()
        cqn = cv.take(3 * S * 2, BF16).rearrange("p (c t) -> p c t", c=3)
        ckvn = cv.take(2 * S * 2, BF16).rearrange("p (c t) -> p c t", c=2)
        tmpf = mixT[:, 6:8, :].rearrange("p a b -> p (a b)")[:, 0:3072].bitcast(F32).rearrange("p (c t) -> p c t", c=3)
        rb = cv.take(512 * 4, F32)
        rope = cv.take(2 * S * 2, BF16).rearrange("p (c t) -> p c t", c=2)
        krope = cv.take(S * 2, BF16)
        t1 = cv.take(512 * 4, F32)
        t2 = rb
        gvec = cv.take(8 * 4, F32)
        TK = [(("mixT", c), i) for c in (6, 7) for i in range(NT)]
        P.dma("sp", rope, cd["c_rope"].rearrange("c p t -> p c t"), "cst", wr=["rope"])
        P.dma("sp", gvec[:, 0:3], g_cq[l].rearrange("c p -> p c"), "cst", wr=["gvec"], allow_slow_non_contiguous=True)
        P.dma("sp", gvec[:, 3:5], g_ckv[l].rearrange("c p -> p c"), "cst", wr=["gvec"], allow_slow_non_contiguous=True)
        P.op("pool", lambda e: e.memset(VA[1][:, :, 0:64], 1.0), wr=[("VA", 1)])

        def latent(c0, nch, dstT, dname, gcol0, inv_n):
            wk = []
            for c in range(nch):
                wk.append(load_win(l, c0 + c * 128, 128))
            for sp in range(NSPAN):
                for c in range(nch):
                    bk, bank = gbank()
                    proj_fm(wk[c][0], wk[c][1], 128, sp, bk, bank)
                    act(tmpf[:, c, :], bank[:, :], AF.Copy, rd=[], wr=[bk] + TK)
                    dve("tensor_tensor", [], TK + [("PT", c)], out=PT[c][:], in0=tmpf[:, c, :], in1=tmpf[:, c, :],
                        op=ALU.mult)
                bk, bank = gbank()
                for c in range(nch):
                    mm(bank[:, :], ones_bf[:], PT[c][:], c == 0, c == nch - 1, rd=["ones_bf", ("PT", c)], wr=[bk])
                act(rb, bank[:, :], AF.Ln, rd=["smalls_c"], wr=[bk, "rb"], scale=inv_n, bias=smalls[:, 0:1])
                act(rb, rb, AF.Exp, rd=[], wr=["rb"], scale=-0.5)
                for c in range(nch):
                    dve("scalar_tensor_tensor", ["gvec"], TK + ["rb", (dname, c, sp)],
                        out=dstT[:, c, sp * 512:(sp + 1) * 512], in0=tmpf[:, c, :], scalar=gvec[:, gcol0 + c:gcol0 + c + 1],
                        in1=rb, op0=ALU.mult, op1=ALU.mult)

        latent(C_CQ, 3, cqn, "cqn", 0, 1.0 / 384)
        latent(C_CKV, 2, ckvn, "ckvn", 3, 1.0 / 256)
        ka = load_win(l, 576, 96)
        kb = load_win(l, 0, 96, src=w_krsw[l].rearrange("(kc p) n -> p kc n", p=128))
        for sp in range(NSPAN):
            bkA, bA = gbank()
            proj_fm(ka[0], ka[1], 96, sp, bkA, bA)
            bkB, bB = gbank()
            proj_fm(kb[0], kb[1], 96, sp, bkB, bB)
            sl = slice(sp * 512, (sp + 1) * 512)
            dve("tensor_tensor", ["rope"], [bkA, "rs"], out=t1[64:96, :], in0=bA[64:96, :], in1=rope[64:96, 0, sl], op=ALU.mult)
            dve("tensor_tensor", ["rope"], [bkB, "rb"], out=t2[64:96, :], in0=bB[64:96, :], in1=rope[64:96, 1, sl], op=ALU.mult)
            dve("tensor_tensor", ["rs", "rb"], [("krope", sp)], out=krope[64:96, sl], in0=t1[64:96, :], in1=t2[64:96, :], op=ALU.add)
        cqn_keys = [("cqn", c, sp) for c in range(3) for sp in range(NSPAN)]
        ckvn_keys = [("ckvn", c, sp) for c in range(2) for sp in range(NSPAN)]
        def prep(h):
            par = h % 2
            QTh, KTh = QTs[par], KTs[par]
            qk_, kk_ = ("QT", par), ("KT", par)
            units = []
            st = {}

            def u_load():
                k1, s1 = wslot()
                st["k1"] = k1
                st["wq"] = s1[:, 0:288].rearrange("p (a b) -> p a b", a=3)
                st["wqs"] = s1[:, 288:576].rearrange("p (a b) -> p a b", a=3)
                st["wkv"] = s1[:, 576:832].rearrange("p (a b) -> p a b", a=2)
                wload(st["wq"], w_uq[l][:, h * 96:(h + 1) * 96].rearrange("(kc p) n -> p kc n", p=128), k1)
                wload(st["wqs"], w_uq_sw[l][:, h * 96:(h + 1) * 96].rearrange("(kc p) n -> p kc n", p=128), k1)
                wload(st["wkv"], w_ukv[l][:, h * 128:(h + 1) * 128].rearrange("(kc p) n -> p kc n", p=128), k1)
                dve("tensor_copy", [("krope", sp) for sp in range(NSPAN)], [kk_], out=KTh[64:96, :], in_=krope[64:96, :])
            units.append(u_load)

            def u_q(sp):
                k1, wq, wqs = st["k1"], st["wq"], st["wqs"]
                sl = slice(sp * 512, (sp + 1) * 512)
                bkA, bA = gbank()
                for c in range(3):
                    mm(bA[0:96, :], wq[:, c, :], cqn[:, c, sl], c == 0, c == 2, rd=[k1] + cqn_keys, wr=[bkA])
                dve("tensor_copy", [], [bkA, qk_], out=QTh[0:64, sl], in_=bA[0:64, :])
                dve("tensor_tensor", ["rope"], [bkA, "t1m"], out=t1[64:96, :], in0=bA[64:96, :], in1=rope[64:96, 0, sl], op=ALU.mult)
                bkB, bB = gbank()
                for c in range(3):
                    mm(bB[0:96, :], wqs[:, c, :], cqn[:, c, sl], c == 0, c == 2, rd=[k1] + cqn_keys, wr=[bkB])
                dve("tensor_tensor", ["rope"], [bkB, "rb"], out=t2[64:96, :], in0=bB[64:96, :], in1=rope[64:96, 1, sl], op=ALU.mult)
                dve("tensor_tensor", ["t1m", "rb"], [qk_], out=QTh[64:96, sl], in0=t1[64:96, :], in1=t2[64:96, :], op=ALU.add)

            def u_k(sp):
                k1, wkv = st["k1"], st["wkv"]
                sl = slice(sp * 512, (sp + 1) * 512)
                bkK, bK = gbank()
                for c in range(2):
                    mm(bK[0:64, :], wkv[:, c, 0:64], ckvn[:, c, sl], c == 0, c == 1, rd=[k1] + ckvn_keys, wr=[bkK])
                dve("tensor_copy", [], [bkK, kk_], out=KTh[0:64, sl], in_=bK[0:64, :])

            def u_v(g):
                k1, wkv = st["k1"], st["wkv"]
                voff = 0 if par == 0 else 64
                bkV, bV = gbank()
                for j in range(4):
                    tt = 4 * g + j
                    for c in range(2):
                        mm(bV[:, j * 64:(j + 1) * 64], ckvn[:, c, tt * 128:(tt + 1) * 128], wkv[:, c, 64:128],
                           (j == 0 and c == 0), (j == 3 and c == 1), rd=[k1] + ckvn_keys, wr=[bkV])
                dve("tensor_copy", [], [bkV, ("VA", par)], out=VA[par][:, 4 * g:4 * g + 4, voff:voff + 64],
                    in_=bV[:, 0:256].rearrange("p (j d) -> p j d", j=4))
            for sp in range(NSPAN):
                units.append(lambda sp=sp: u_q(sp))
                units.append(lambda sp=sp: u_k(sp))
            for g in range(4):
                units.append(lambda g=g: u_v(g))
            return units

        for u in prep(0):
            u()
        for h in range(6):
            par = h % 2
            cch = h // 2
            nxt = prep(h + 1) if h + 1 < 6 else []
            run_groups([make_group(("QT", par), ("KT", par), KTs[par], ("VA", par), VA[par], 0, 96, 96 ** -0.5, sp, "causal",
                                   (lambda okey, ob, par=par, cch=cch, sp=sp: post_simple(okey, ob, par, cch, sp)),
                                   qtile=QTs[par])
                        for sp in range(NSPAN)], fillers=nxt)

    def fox_phase(l):
        cv = Carver()
        big = cv.take(2 * S * 4, F32)
        nl = big[:, 0:S]
        a8 = big[:, S:2 * S]
        rr = nl
        Vall = big.bitcast(BF16).rearrange("p (t b d) -> p t b d", t=NT, b=8)
        CH = cv.take(3 * S * 2, BF16).rearrange("p (c t) -> p c t", c=3)
        bvec = cv.take(8 * 4, F32)
        P.dma("sp", bvec[0:6, 0:1], b_forget[l], "cst", wr=["bvec"])
        dve("tensor_scalar", ["bvec"], ["bvec"], out=bvec[0:6, 1:2], in0=bvec[0:6, 0:1], scalar1=-1.0, scalar2=None, op0=ALU.mult)
        wf = load_win(l, C_FF, 6)
        for sp in range(NSPAN):
            sl = slice(sp * 512, (sp + 1) * 512)
            bk, bank = gbank()
            proj_fm(wf[0], wf[1], 6, sp, bk, bank)
            act(nl[0:6, sl], bank[0:6, :], AF.Exp, rd=["bvec"], wr=[bk, "nl"], scale=-1.0, bias=bvec[0:6, 1:2])
            act(nl[0:6, sl], nl[0:6, sl], AF.Ln, rd=["smalls_c"], wr=["nl"], bias=smalls[0:6, 1:2])
        dve("tensor_tensor_scan", ["nl", "smalls_c"], ["a8"], out=a8[0:6, :], data0=smalls[0:6, 1:2].to_broadcast([6, S]), data1=nl[0:6, :], initial=0.0,
            op0=ALU.mult, op1=ALU.add)
        dve("tensor_scalar", ["a8"], ["a8"], out=a8[0:6, :], in0=a8[0:6, :], scalar1=8.0, scalar2=None, op0=ALU.mult)
        dve("tensor_copy", ["a8"], ["CH"], out=CH[0:6, 0, :], in_=a8[0:6, :])
        dve("tensor_tensor", ["a8", "CH"], ["nl"], out=rr[0:6, :], in0=a8[0:6, :], in1=CH[0:6, 0, :], op=ALU.subtract)
        dve("tensor_copy", ["nl"], ["CH"], out=CH[0:6, 1, :], in_=rr[0:6, :])
        dve("tensor_tensor", ["CH"], ["nl"], out=rr[0:6, :], in0=rr[0:6, :], in1=CH[0:6, 1, :], op=ALU.subtract)
        dve("tensor_copy", ["nl"], ["CH"], out=CH[0:6, 2, :], in_=rr[0:6, :])
        P.op("pool", lambda e: e.memset(Vall[:, :, 0, :], 1.0), wr=["nl", "a8", "Vall"])
        P.op("pool", lambda e: e.memset(Vall[:, :, 7, :], 1.0), wr=["Vall"])
        wp_i[0] = (wp_i[0] + 3) // 4 * 4
        vkeys = []
        for _ in range(3):
            k_, _s = wslot()
            vkeys.append(k_)
        wv_all = wpool_t[:, 0:3072].rearrange("p (a b) -> p a b", a=8)
        for i_, k_ in enumerate(vkeys):
            pass
        P.dma("pool", wv_all, w_in_view(l, C_FV, 384), "w0", wr=vkeys)
        for tt in range(NT):
            bkV, bV = gbank()
            for kc in range(8):
                mm(bV[:, 0:384], xT[:, kc, tt * 128:(tt + 1) * 128], wv_all[:, kc, :], kc == 0, kc == 7,
                   rd=vkeys + [("xT", tt)], wr=[bkV])
            act(Vall[:, tt, 1:7, :], bV[:, 0:384].rearrange("p (h d) -> p h d", h=6), AF.Copy, rd=[], wr=[bkV, "Vall"])

        def prep(h):
            par = h % 2
            QTh, KTh = QTs[par], KTs[par]
            qk_, kk_ = ("QT", par), ("KT", par)
            st = {}
            units = []

            def u_load():
                key, slot = wslot()
                v = slot[:, :].rearrange("p (a b) -> p a b", a=8)
                wload(v[:, :, 0:64], w_in_view(l, C_FQ + 64 * h, 64), key)
                wload(v[:, :, 64:128], w_in_view(l, C_FK + 64 * h, 64), key)
                st["w"] = (key, v)
                for r in range(3):
                    P.dma("sp", QTh[64 + r:65 + r, :], CH[h:h + 1, r, :], "aug", rd=["CH"], wr=[qk_], newgroup=(r == 0))
                    P.dma("sp", KTh[67 + r:68 + r, :], CH[h:h + 1, r, :], "aug", rd=["CH"], wr=[kk_], newgroup=False)
                P.dma("sp", QTh[67:70, :], cd["c_ones"], "aug", wr=[qk_], newgroup=False)
                P.dma("sp", KTh[64:67, :], cd["c_negones"], "aug", wr=[kk_], newgroup=False)
            units.append(u_load)

            def u_qk(sp):
                w = st["w"]
                sl = slice(sp * 512, (sp + 1) * 512)
                bk, bank = gbank()
                proj_fm(w[0], w[1], 128, sp, bk, bank)
                dve("tensor_copy", [], [bk, qk_], out=QTh[0:64, sl], in_=bank[0:64, :])
                dve("tensor_copy", [], [bk, kk_], out=KTh[0:64, sl], in_=bank[64:128, :])
            def u_v():
                voff = 0 if par == 0 else 64
                P.op("pool", lambda e: e.tensor_copy(out=VA[par][:, :, voff:voff + 64], in_=Vall[:, :, h + 1, :]),
                     rd=["Vall"], wr=[("VA", par)])
            for sp in range(NSPAN):
                units.append(lambda sp=sp: u_qk(sp))
            units.append(u_v)
            return units

        for u in prep(0):
            u()
        for h in range(6):
            par = h % 2
            cch = 3 + h // 2
            nxt = prep(h + 1) if h + 1 < 6 else []
            run_groups([make_group(("QT", par), ("KT", par), KTs[par], ("VA", par), VA[par], 0, 70, 0.125, sp, "causal",
                                   (lambda okey, ob, par=par, cch=cch, sp=sp: post_simple(okey, ob, par, cch, sp)),
                                   qtile=QTs[par])
                        for sp in range(NSPAN)], fillers=nxt)

    def nsa_phase(l):
        cv = Carver()
        SELB = cv.take(S * 2, BF16)
        KW = cv.take(S * 2, BF16)
        KC = cv.take(128 * 2, BF16)
        VC = cv.take(128 * 2, BF16)
        GT = cv.take(S * 2, BF16)
        maskcmp = cv.take(S * 2, BF16)
        kvT = cv.take(S * 2, BF16)
        imp = cv.take(NT * 32 * 4, F32).rearrange("p (a b) -> p a b", a=NT)
        impm = cv.take(NT * 32 * 4, F32).rearrange("p (a b) -> p a b", a=NT)
        keepadd = cv.take(2 * NT * 32 * 4, F32).rearrange("p (k a b) -> p k a b", k=2, a=NT)
        h1 = cv.take(2 * 128 * 2, BF16).rearrange("p (a b) -> p a b", a=2)
        gsel = cv.take(12 * 128 * 2, BF16).rearrange("p (a b) -> p a b", a=12)
        ovl = cv.take(64 * 2, BF16)
        pe2 = cv.take(32 * 2, BF16)
        acc = cv.take(512 * 4, F32)
        tmp = cv.take(512 * 4, F32)
        selbf = kvT[:, 0:1536].rearrange("p (a b) -> p a b", a=NT)
        fac = cv.take(512 * 4, F32)
        top8 = cv.take(NT * 8 * 4, F32).rearrange("p (a b) -> p a b", a=NT)
        r4 = cv.take(8 * 4, F32)
        P.dma("sp", KT[64:96, :], cd["c_onehot"], "cst", wr=[("KT", 0)])
        P.op("pool", lambda e: e.memset(KW[64:96, :], 0.0), wr=["KW"])
        P.op("pool", lambda e: e.memset(KC[64:96, :], 0.0), wr=["KC"])
        P.dma("sp", KT[96:100, :], cd["c_alibi_k"][0], "cst", wr=[("KT", 0)])
        P.dma("sp", KW[96:100, :], cd["c_alibi_k"][0], "cst", wr=["KW"])
        P.dma("sp", KC[96:100, :], cd["c_alibi_k"][1][:, 0:128], "cst", wr=["KC"])
        P.dma("sp", maskcmp, cd["c_maskcmp"], "cst", wr=["maskcmp"])
        P.dma("sp", keepadd, cd["c_keepadd"], "cst", wr=["keepadd"])
        P.dma("sp", gsel[0:44, :, :], cd["c_gsel"], "cst", wr=["gsel"])
        P.dma("sp", ovl[:, 0:33], cd["c_ovl"], "cst", wr=["ovl"])
        P.op("pool", lambda e: e.memset(VA[1][:, :, 64:128], 1.0), wr=[("VA", 1)])
        P.op("pool", lambda e: e.memset(VC[:, 64:128], 1.0), wr=["VC"])
        P.dma("pool", pe2[0:64, :], cmp_posT[l][0], "cst2", wr=["pe2"])
        P.dma("pool", pe2[64:128, :], cmp_posT[l][1], "cst2", wr=["pe2"])

        wkv = load_win(l, C_KCMP, 128)
        kkey, kslot = wslot()
        wkk = kslot[:, :].rearrange("p (a b) -> p a b", a=8)
        wload(wkk[:, :, 0:64], w_in_view(l, C_KSLC, 64), kkey)
        wload(wkk[:, :, 64:128], w_in_view(l, C_KWIN, 64), kkey)
        wg = load_win(l, C_GATE, 12)
        for sp in range(NSPAN):
            sl = slice(sp * 512, (sp + 1) * 512)
            bk, bank = gbank()
            proj_fm(wkv[0], wkv[1], 128, sp, bk, bank)
            act(kvT[:, sl], bank[:, :], AF.Copy, rd=[], wr=[bk, "kvT"])
            bk, bank = gbank()
            proj_fm(kkey, wkk, 128, sp, bk, bank)
            act(KT[0:64, sl], bank[0:64, :], AF.Copy, rd=[], wr=[bk, ("KT", 0)])
            dve("tensor_copy", [], [bk, "KW"], out=KW[0:64, sl], in_=bank[64:128, :])
            bk, bank = gbank()
            proj_fm(wg[0], wg[1], 12, sp, bk, bank)
            act(tmp[0:12, :], bank[0:12, :], AF.Sigmoid, rd=[], wr=[bk, "tmp"])
            dve("tensor_copy", ["tmp"], ["GT"], out=GT[0:12, sl], in_=tmp[0:12, :])
            dve("tensor_tensor", ["tmp"], ["GT"], out=GT[32:44, sl], in0=tmp[0:12, :], in1=GT[0:12, sl], op=ALU.subtract)
        vkey, vslot = wslot()
        wvv = vslot[:, :].rearrange("p (a b) -> p a b", a=8)
        wload(wvv[:, :, 0:64], w_in_view(l, C_VSLC, 64), vkey)
        wload(wvv[:, :, 64:128], w_in_view(l, C_VWIN, 64), vkey)
        for g in range(4):
            bkV, bV = gbank()
            for j in range(4):
                tt = 4 * g + j
                for kc in range(8):
                    mm(bV[:, j * 128:(j + 1) * 128], xT[:, kc, tt * 128:(tt + 1) * 128], wvv[:, kc, :],
                       (j == 0 and kc == 0), (j == 3 and kc == 7), rd=[vkey, ("xT", tt)], wr=[bkV])
            bv4 = bV[:, :].rearrange("p (j d) -> p j d", j=4)
            act(VA[0][:, 4 * g:4 * g + 4, 0:64], bv4[:, :, 0:64], AF.Copy, rd=[], wr=[bkV, ("VA", 0)])
            act(VA[1][:, 4 * g:4 * g + 4, 0:64], bv4[:, :, 64:128], AF.Copy, rd=[], wr=[bkV, ("VA", 1)])
        w1s = []
        for i in range(4):
            key, slot = wslot()
            v = slot[:, :].rearrange("p (a b) -> p a b", a=8)
            for kv in range(2):
                src = cmp_w1[l][kv].rearrange("(l d) j -> d l j", d=64)[:, 8 * i:8 * i + 8, :]
                wload(v[64 * kv:64 * kv + 64, :, :], src, key)
            w1s.append((key, v))
        key2 = "w2cmp"
        w2t = cv.take(128 * 2, BF16)
        w2k = w2t[:, 0:64]
        w2v = w2t[:, 64:128]
        P.dma("pool", w2k, cmp_w2[l][0], "cst2", wr=[key2])
        P.dma("pool", w2v, cmp_w2[l][1], "cst2", wr=[key2], newgroup=False)
        for kv in range(2):
            pr = slice(64 * kv, 64 * kv + 64)
            bk, bank = gbank()
            for li in range(32):
                wkey, wvw_ = w1s[li // 8]
                mm(bank[:, 0:127], wvw_[pr, li % 8, :], kvT[pr, li:li + 16 * 126 + 1:16], li == 0 and True, False,
                   rd=[wkey, "kvT"], wr=[bk])
                mm(bank[:, 127:128], wvw_[pr, li % 8, :], pe2[pr, li:li + 1], False, li == 31, rd=[wkey, "pe2"], wr=[bk])
            act(smalls[:, 2 + kv:3 + kv], bank[:, 127:128], AF.Copy, rd=[], wr=[bk, ("sm", 2 + kv)])
            act(h1[:, kv, 0:127], bank[:, 0:127], AF.Silu, rd=[("sm", 2 + kv)], wr=[bk, ("h1", kv)], bias=smalls[:, 2 + kv:3 + kv])
        bk, bank = gbank()
        mm(bank[0:64, 0:127], w2k, h1[:, 0, 0:127], True, True, rd=[key2, ("h1", 0)], wr=[bk])
        act(KC[0:64, 0:127], bank[0:64, 0:127], AF.Copy, rd=[], wr=[bk, "KC"])
        bk, bank = gbank()
        mm(bank[0:127, 0:64], h1[:, 1, 0:127], w2v, True, True, rd=[key2, ("h1", 1)], wr=[bk])
        act(VC[0:127, 0:64], bank[0:127, 0:64], AF.Copy, rd=[], wr=[bk, "VC"])

        def load_q_units(h, with_sel):
            qs = h % 2
            QTh = QTs[qs]
            st = {}
            units = []

            def u0():
                st["wq"] = load_win(l, C_NQ + 64 * h, 64)
                P.dma("sp", QTh[96:100, :], cd["c_alibi_q"][h], "aug", wr=[("QT", qs)])
                if with_sel:
                    dve("tensor_copy", ["SELB"], [("QT", qs)], out=QTh[64:96, :], in_=SELB[64:96, :])
            units.append(u0)

            def u1(sp):
                wq = st["wq"]
                sl = slice(sp * 512, (sp + 1) * 512)
                bk, bank = gbank()
                proj_fm(wq[0], wq[1], 64, sp, bk, bank)
                dve("tensor_copy", [], [bk, ("QT", qs)], out=QTh[0:64, sl], in_=bank[0:64, :])
            for sp in range(NSPAN):
                units.append(lambda sp=sp: u1(sp))
            return units

        def load_q(h, with_sel=False):
            for u in load_q_units(h, with_sel):
                u()

        def cmp_scores(sp, qs):
            sl = slice(sp * 512, (sp + 1) * 512)
            skey, sbk = sbank()
            mm(sbk[0:127, :], KC[0:100, 0:127], QTs[qs][0:100, sl], True, False, rd=["KC", ("QT", qs)], wr=[skey])
            mm(sbk[0:127, :], ident[0:127, 0:127], maskcmp[0:127, sl], False, True, rd=["ident", "maskcmp"], wr=[skey])
            pkey, pt = ptile()
            act(pt[0:127, :], sbk[0:127, :], AF.Exp, rd=[], wr=[skey, pkey], scale=0.125)
            return pkey, pt

        load_q(0)
        for h in range(4):
            if h + 1 < 4:
                load_q(h + 1)
            for sp in range(NSPAN):
                pkey, pt = cmp_scores(sp, h % 2)
                bk, bank = gbank()
                for j in range(4):
                    mm(bank[:, j * 33:(j + 1) * 33], pt[0:127, j * 128:(j + 1) * 128], ovl[0:127, 0:33], j == 0, j == 3,
                       rd=[pkey, "ovl"], wr=[bk])
                bv = bank[:, 0:132].rearrange("p (j c) -> p j c", j=4)
                dve("tensor_scalar_max", [], [bk, "r4"], out=r4[:, 0:4], in0=bv[:, :, 32], scalar1=1e-30)
                dve("reciprocal", [], ["r4"], out=r4[:, 0:4], in_=r4[:, 0:4])
                r4b = r4[:, 0:4].unsqueeze(2).to_broadcast([128, 4, 32])
                if h == 0:
                    dve("tensor_tensor", ["r4"], [bk, ("imp", sp)], out=imp[:, 4 * sp:4 * sp + 4, :], in0=bv[:, :, 0:32], in1=r4b,
                        op=ALU.mult)
                else:
                    tv = tmp[:, 0:128].rearrange("p (a b) -> p a b", a=4)
                    dve("tensor_tensor", ["r4"], [bk, "tmp"], out=tv, in0=bv[:, :, 0:32], in1=r4b, op=ALU.mult)
                    dve("tensor_tensor", ["tmp"], [("imp", sp)], out=imp[:, 4 * sp:4 * sp + 4, :], in0=imp[:, 4 * sp:4 * sp + 4, :],
                        in1=tv, op=ALU.add)
        impk = [("imp", sp) for sp in range(NSPAN)]
        dve("tensor_tensor", impk + ["keepadd"], ["impm"], out=impm[:, :, :], in0=imp[:, :, :], in1=keepadd[:, 0, :, :], op=ALU.mult)
        dve("tensor_tensor", ["keepadd"], ["impm"], out=impm[:, :, :], in0=impm[:, :, :], in1=keepadd[:, 1, :, :], op=ALU.add)
        for tt in range(NT):
            dve("max", ["impm"], [("top8", tt)], out=top8[:, tt, :], in_=impm[:, tt, :])
        for tt in range(NT):
            dve("tensor_scalar", [("top8", tt)], ["impm"], out=impm[:, tt, :], in0=impm[:, tt, :], scalar1=top8[:, tt, 7:8],
                scalar2=None, op0=ALU.is_ge)
        dve("tensor_scalar", [], ["impm", "kvT", "selbf"], out=selbf[:, :, 64:96], in0=impm[:, :, :], scalar1=-1.0, scalar2=-NEG,
            op0=ALU.add, op1=ALU.mult)
        for half in range(2):
            bk, bank = gbank()
            bv = bank[:].bitcast(BF16)
            for j in range(8):
                tt = 8 * half + j
                P.op("pe", lambda e, j=j, tt=tt, bv=bv: e.transpose(bv[0:96, j * 128:(j + 1) * 128], selbf[:, tt, :], ident[:]),
                     rd=["selbf", "ident"], wr=[bk])
            act(SELB[64:96, half * 1024:(half + 1) * 1024], bv[64:96, :], AF.Copy, rd=[], wr=[bk, "SELB"])
        dbg_dump("d_selb", SELB[64:96, :], ["SELB"])
        dve("memset", [], ["kvT", "fac"], fac[0:64, 0:1], 0.0)
        def cmp_group(sp, post, qs):
            sl = slice(sp * 512, (sp + 1) * 512)
            t = dict(lhsT=KC[0:100, 0:127], rhs=QTs[qs][0:100, sl], c0=0, c1=512,
                     masks=[(0, 512, ident[0:127, 0:127], maskcmp[0:127, sl], ["ident", "maskcmp"])], prow=127,
                     va=VC[0:127, :], rdk=["KC", ("QT", qs)], vak="VC", scale=0.125)
            return dict(tasks=[t], post=post)

        load_q(0, True)
        for h in range(4):
            par = h % 2
            cch = 6 + h // 2
            qs = h % 2

            def branch_post(okey, ob, b, sp, h=h, par=par, cch=cch):
                sl = slice(sp * 512, (sp + 1) * 512)
                gk, gb = gbank()
                mm(gb[:, :], gsel[0:44, 3 * h + b, :], GT[0:44, sl], True, True, rd=["gsel", "GT"], wr=[gk])
                if b == 0:
                    act(rs[0:64, :], ob[64:128, :], AF.Ln, rd=["smalls_c"], wr=[okey, "rs"], bias=smalls[0:64, 4:5])
                else:
                    act(rs[0:64, :], ob[64:128, :], AF.Ln, rd=[], wr=[okey, "rs"])
                act(rs[0:64, :], rs[0:64, :], AF.Exp, rd=[], wr=["rs"], scale=-1.0)
                dve("tensor_tensor", ["rs"], [gk, "fac"], out=fac[0:64, :], in0=gb[0:64, :], in1=rs[0:64, :], op=ALU.mult)
                if b == 0:
                    dve("tensor_tensor", ["fac"], [okey, "acc"], out=acc[0:64, :], in0=ob[0:64, :], in1=fac[0:64, :], op=ALU.mult)
                else:
                    dve("tensor_tensor", ["fac"], [okey, "tmp"], out=tmp[0:64, :], in0=ob[0:64, :], in1=fac[0:64, :], op=ALU.mult)
                    P.op("pool", lambda e: e.tensor_tensor(out=acc[0:64, :], in0=acc[0:64, :], in1=tmp[0:64, :], op=ALU.add),
                         rd=["tmp"], wr=["acc"])
                if b == 2:
                    a_ = 64 * par
                    dve("tensor_copy", ["acc"], span_keys(("mixT", cch), sp), out=mixT[a_:a_ + 64, cch, sl], in_=acc[0:64, :])

            groups = []
            for sp in range(NSPAN):
                groups.append(cmp_group(sp, (lambda okey, ob, sp=sp: branch_post(okey, ob, 0, sp)), qs))
                groups.append(make_group(("QT", qs), ("KT", 0), KT, ("VA", 0), VA[0], 0, 100, 0.125, sp, "causal",
                                         (lambda okey, ob, sp=sp: branch_post(okey, ob, 1, sp)), qtile=QTs[qs]))
                groups.append(make_group(("QT", qs), "KW", KW, ("VA", 1), VA[1], 0, 100, 0.125, sp, "window",
                                         (lambda okey, ob, sp=sp: branch_post(okey, ob, 2, sp)), qtile=QTs[qs]))
            run_groups(groups, fillers=(load_q_units(h + 1, True) if h + 1 < 4 else []))

    def post_phase(l, sq):
        cv = Carver()
        lnp = cv.take(2 * D * 4, F32).rearrange("p (a b) -> p a b", a=2)
        stats = cv.take(12 * 4, F32)
        mv = cv.take(8 * 4, F32)
        sc = cv.take(8 * 4, F32)
        tmps = [cv.take(512 * 4, F32), cv.take(512 * 4, F32)]
        xb2 = cv.take(D * 2, BF16)
        rwf = cv.take(64 * 4, F32).rearrange("p (a b) -> p a b", a=8)
        rwh = cv.take(2 * 64 * 2, BF16).rearrange("p (k a b) -> p k a b", k=2, a=8)
        lg = cv.take(128 * 4, F32).rearrange("p (a b) -> p a b", a=NT)
        eg = cv.take(128 * 4, F32).rearrange("p (a b) -> p a b", a=NT)
        mk = cv.take(128 * 4, F32).rearrange("p (a b) -> p a b", a=NT)
        gates = cv.take(128 * 4, F32).rearrange("p (a b) -> p a b", a=NT)
        m8 = cv.take(128 * 4, F32).rearrange("p (a b) -> p a b", a=NT)
        sm16 = cv.take(64 * 4, F32)
        moe = (l % 2 == 1)
        hT = mixT
        wmod[0] = 8

        def load_ln(which):
            P.dma("sp", lnp, ln_gb[l][2 * which:2 * which + 2, :].partition_broadcast(128), "cst", wr=["lnp"])

        xbs = [cv.take(D * 2, BF16), cv.take(D * 2, BF16)]
        xb2s = [xb2, cv.take(D * 2, BF16)]
        smallv = cv.take(64 * 4, F32)

        def ln_a(tt):
            pq = tt % 2
            xk = ("xres", tt)
            st = smallv[:, 12 * pq:12 * pq + 12]
            mv_ = smallv[:, 24 + 2 * pq:26 + 2 * pq]
            rst = smallv[:, 28 + pq:29 + pq]
            kst, kmv, krs = ("st", pq), ("mv", pq), ("rst", pq)
            dve("bn_stats", [], [xk, kst], out=st[:, 0:6], in_=xres[:, tt, 0:512])
            dve("bn_stats", [], [xk, kst], out=st[:, 6:12], in_=xres[:, tt, 512:1024])
            dve("bn_aggr", [], [kst, kmv], out=mv_, in_=st)
            act(rst, mv_[:, 1:2], AF.Sqrt, rd=["smalls_c"], wr=[kmv, krs], bias=smalls[:, 0:1])

        def ln_b(tt, final, make_lo):
            pq = tt % 2
            xk = ("xres", tt)
            xt_ = xres[:, tt, :]
            mv_ = smallv[:, 24 + 2 * pq:26 + 2 * pq]
            rst = smallv[:, 28 + pq:29 + pq]
            kst, kmv, krs = ("st", pq), ("mv", pq), ("rst", pq)
            dve("reciprocal", [], [krs], out=rst, in_=rst)
            dve("scalar_tensor_tensor", ["lnp", kmv], [xk], out=xt_, in0=xt_, scalar=mv_[:, 0:1], in1=lnp[:, 0, :],
                op0=ALU.subtract, op1=ALU.mult)
            dve("scalar_tensor_tensor", ["lnp", krs], [xk], out=xt_, in0=xt_, scalar=rst, in1=lnp[:, 1, :],
                op0=ALU.mult, op1=ALU.add)
            if final:
                P.dma("sp", out_d[sq, tt * 128:(tt + 1) * 128, :], xt_, "out%d" % (tt % 4), rd=[xk])
                return
            xb = xbs[pq]
            xbk = ("xb", pq)
            act(xb[:], xt_, AF.Copy, rd=[xk], wr=[xbk])
            if make_lo:
                x2 = xb2s[pq]
                x2k = ("xb2", pq)
                dve("tensor_tensor", [xk, xbk], [x2k], out=x2, in0=xt_, in1=xb[:], op=ALU.subtract)

        def ln_c(tt, final, make_lo):
            if final:
                return
            pq = tt % 2
            xb = xbs[pq]
            xbk = ("xb", pq)
            if make_lo:
                x2 = xb2s[pq]
                x2k = ("xb2", pq)
            transpose_to_xT(tt, xbk, xb, xT, "xT")
            if make_lo:
                bk, bank = gbank()
                bv = bank[:].bitcast(BF16)
                for c in range(8):
                    P.op("pe", lambda e, c=c, bv=bv, x2=x2: e.transpose(bv[:, c * 128:(c + 1) * 128], x2[:, c * 128:(c + 1) * 128], ident[:]),
                         rd=[x2k, "ident"], wr=[bk])
                act(mixT[:, :, tt * 128:(tt + 1) * 128], bv.rearrange("p (c t) -> p c t", c=8), AF.Copy,
                    rd=[], wr=[bk] + [(("mixT", c), tt) for c in range(8)])

        load_ln(0)
        wo = []
        for c in range(8):
            key, slot = wslot()
            wload(slot[:, :], w_out[l][c * 128:(c + 1) * 128, :], key)
            wo.append((key, slot))
        pb_i = [0]

        def pbank():
            i = pb_i[0] % 6
            pb_i[0] += 1
            return ("ps", i), psb[i]

        def outproj(tt):
            for half in range(2):
                bk, bank = pbank()
                for c in range(8):
                    mm(bank[:, :], mixT[:, c, tt * 128:(tt + 1) * 128], wo[c][1][:, half * 512:(half + 1) * 512], c == 0, c == 7,
                       rd=[wo[c][0], (("mixT", c), tt)], wr=[bk])
                xs_ = xres[:, tt, half * 512:(half + 1) * 512]
                dve("scalar_tensor_tensor", [], [bk, ("xres", tt)], out=xs_, in0=xs_, scalar=ALPHA, in1=bank[:, :],
                    op0=ALU.mult, op1=ALU.add)

        outproj(0)
        outproj(1)
        outproj(2)
        ln_a(0)
        for tt in range(NT):
            if tt + 3 < NT:
                outproj(tt + 3)
            if tt + 1 < NT:
                ln_a(tt + 1)
            ln_b(tt, False, moe)
            if tt >= 1:
                ln_c(tt - 1, False, moe)
        ln_c(NT - 1, False, moe)
        load_ln(1)

        ffn_groups = []
        wa_i = [0]
        wb_i = [0]

        def wslotA():
            i = wa_i[0] % 4
            wa_i[0] += 1
            wgen[i] = True
            return ("wp", i), wpool[i]

        def wslotB():
            i = 4 + wb_i[0] % 4
            wb_i[0] += 1
            wgen[i] = True
            return ("wp", i), wpool[i]

        def expert(w1d, w3d, w2d, nchunk, gate_e, first_scale):
            gl = [list(range(i, min(i + 4, nchunk))) for i in range(0, nchunk, 4)]
            for gi, grp in enumerate(gl):
                ffn_groups.append(dict(w1d=w1d, w3d=w3d, w2d=w2d, grp=grp, gate_e=gate_e, scale=(first_scale and gi == 0)))

        def run_ffn(tail, tail2):
            ng = len(ffn_groups)
            for g, G in enumerate(ffn_groups):
                grp = G["grp"]
                hb = 4 * (g % 2)
                w2s = []

                def load_w2():
                    for ci, c in enumerate(grp):
                        key, slot = wslotB()
                        wload(slot[:, :], G["w2d"][c * 128:(c + 1) * 128, :], key)
                        w2s.append((key, slot))
                for ci, c in enumerate(grp):
                    ws = []
                    for wd in (G["w1d"], G["w3d"]):
                        key, slot = wslotA()
                        v = slot[:, :].rearrange("p (a b) -> p a b", a=8)
                        wload(v, wd[:, c * 128:(c + 1) * 128].rearrange("(kc p) n -> p kc n", p=128), key)
                        ws.append((key, v))
                    if ci == min(1, len(grp) - 1):
                        load_w2()
                    (k1, v1), (k3, v3) = ws
                    for sp in range(NSPAN):
                        sl = slice(sp * 512, (sp + 1) * 512)
                        b1k, b1 = sbank()
                        proj_fm(k1, v1, 128, sp, b1k, b1)
                        b3k, b3 = gbank()
                        proj_fm(k3, v3, 128, sp, b3k, b3)
                        tsp = tmps[sp % 2]
                        tk = ("tmps", sp % 2)
                        act(tsp, b1[:, :], AF.Silu, rd=[], wr=[b1k, tk])
                        dve("tensor_tensor", [tk], [b3k] + span_keys(("mixT", hb + ci), sp), out=hT[:, hb + ci, sl], in0=b3[:, :],
                            in1=tsp, op=ALU.mult)
                gate_e = G["gate_e"]
                for tt in range(NT):
                    for half in range(2):
                        bk, bank = obank()
                        for ci in range(len(grp)):
                            mm(bank[:, :], hT[:, hb + ci, tt * 128:(tt + 1) * 128], w2s[ci][1][:, half * 512:(half + 1) * 512],
                               ci == 0, ci == len(grp) - 1, rd=[w2s[ci][0], (("mixT", hb + ci), tt)], wr=[bk])
                        xs_ = xres[:, tt, half * 512:(half + 1) * 512]
                        if gate_e is None:
                            if G["scale"]:
                                dve("scalar_tensor_tensor", [], [bk, ("xres", tt)], out=xs_, in0=xs_, scalar=ALPHA, in1=bank[:, :],
                                    op0=ALU.mult, op1=ALU.add)
                            else:
                                dve("tensor_tensor", [], [bk, ("xres", tt)], out=xs_, in0=xs_, in1=bank[:, :], op=ALU.add)
                        else:
                            dve("scalar_tensor_tensor", ["gates"], [bk, ("xres", tt)], out=xs_, in0=bank[:, :],
                                scalar=gates[:, tt, gate_e:gate_e + 1], in1=xs_, op0=ALU.mult, op1=ALU.add)
                    if g == ng - 1:
                        if tt >= 1:
                            ln_a(tt - 1)
                        if tt >= 2:
                            tail(tt - 2)
                        if tt >= 3:
                            tail2(tt - 3)
            ln_a(NT - 1)
            tail(NT - 2)
            tail2(NT - 3)
            tail(NT - 1)
            tail2(NT - 2)
            tail2(NT - 1)

        if not moe:
            j = l // 2
            expert(ffn_w1[j], ffn_w3[j], ffn_w2[j], D_FF // 128, None, True)
            run_ffn(lambda tt: ln_b(tt, l == nlayers - 1, False), lambda tt: ln_c(tt, l == nlayers - 1, False))
        else:
            j = l // 2
            P.dma("sp", rwf, router_w[j].rearrange("(kc p) e -> p kc e", p=128), "cst", wr=["rwf"])
            dve("tensor_copy", ["rwf"], ["rwh"], out=rwh[:, 0, :, :], in_=rwf)
            dve("tensor_tensor", ["rwf"], ["rwh"], out=rwh[:, 1, :, :], in0=rwf, in1=rwh[:, 0, :, :], op=ALU.subtract)
            bk, bank = gbank()
            n = 0
            for tt in range(NT):
                tsl = slice(tt * 128, (tt + 1) * 128)
                combos = [(xT, 0, ("xT", tt)), (mixT, 0, None), (xT, 1, ("xT", tt))]
                for ci_, (src, wi, key) in enumerate(combos):
                    for kc in range(8):
                        rdk = ["rwh"] + ([key] if key else [(("mixT", c), tt) for c in range(8)])
                        mm(bank[:, tt * 8:(tt + 1) * 8], src[:, kc, tsl], rwh[:, wi, kc, :], n == 0, False, rd=rdk, wr=[bk])
                        n += 1
            act(lg[:, :, :], bank[:, 0:128].rearrange("p (a b) -> p a b", a=NT), AF.Copy, rd=[], wr=[bk, "lg"])
            for tt in range(NT):
                dve("max", ["lg"], ["m8"], out=m8[:, tt, :], in_=lg[:, tt, :])
            dve("tensor_scalar", ["m8"], ["sm16"], out=sm16[:, 0:16], in0=m8[:, :, 0], scalar1=-1.0, scalar2=None, op0=ALU.mult)
            for tt in range(NT):
                act(eg[:, tt, :], lg[:, tt, :], AF.Exp, rd=["lg", "sm16"], wr=["eg"], bias=sm16[:, tt:tt + 1])
                dve("tensor_scalar", ["lg", "m8"], ["mk"], out=mk[:, tt, :], in0=lg[:, tt, :], scalar1=m8[:, tt, 1:2], scalar2=None,
                    op0=ALU.is_ge)
            dve("tensor_tensor", ["mk"], ["eg"], out=eg[:, :, :], in0=eg[:, :, :], in1=mk[:, :, :], op=ALU.mult)
            dve("tensor_reduce", ["eg"], ["sm16"], out=sm16[:, 16:32], in_=eg[:, :, :], axis=mybir.AxisListType.X, op=ALU.add)
            dve("reciprocal", [], ["sm16"], out=sm16[:, 16:32], in_=sm16[:, 16:32])
            dve("tensor_tensor", ["eg", "sm16"], ["gates"], out=gates[:, :, :], in0=eg[:, :, :],
                in1=sm16[:, 16:32].unsqueeze(2).to_broadcast([128, NT, 8]), op=ALU.mult)
            for tt in range(NT):
                dve("tensor_scalar", [], [("xres", tt)], out=xres[:, tt, :], in0=xres[:, tt, :], scalar1=ALPHA, scalar2=None,
                    op0=ALU.mult)
            for e_ in range(N_EXP):
                expert(moe_w1[j][e_], moe_w3[j][e_], moe_w2[j][e_], D_FFE // 128, e_, False)
            run_ffn(lambda tt: ln_b(tt, l == nlayers - 1, False), lambda tt: ln_c(tt, l == nlayers - 1, False))

    def run_all():
        for sq in range(nseq):
            load_x(sq)
            for l in range(nlayers):
                for ph in (mla_phase, fox_phase, nsa_phase):
                    P.fence(lambda e: e.memset(smalls[:, 60:61], 0.0))
                    ph(l)
                P.fence(lambda e: e.memset(smalls[:, 60:61], 0.0))
                post_phase(l, sq)
                wmod[0] = 4

    def load_x(sq):
        for tt in range(NT):
            P.dma("sp", xres[:, tt, :], x_d[sq, tt * 128:(tt + 1) * 128, :], "xin%d" % (tt % 4), wr=[("xres", tt)])
            act(xb_ld[:], xres[:, tt, :], AF.Copy, rd=[("xres", tt)], wr=["xb_ld"])
            transpose_to_xT(tt, "xb_ld", xb_ld, xT, "xT")

    stage = dbg.get("stage", (None, None))[0] if False else None
    PH = dict(mla=mla_phase, fox=fox_phase)
    return nc, P, es, locals()


def finish(nc, P, es, final_waits):
    P.emit(es, final_waits)
    es.close()
    return nc


def prep_weights(inp):
    d = {}
    w = inp["w_in"]
    d["w_in"] = w
    kr = w[:, :, 640:672]
    d["w_krsw"] = np.ascontiguousarray(np.concatenate([w[:, :, 576:640], kr[:, :, 16:32], kr[:, :, 0:16]], -1))
    d["b_forget"] = np.ascontiguousarray(inp["b_forget"].reshape(2, 6, 1))
    d["g_cq"] = np.ascontiguousarray(inp["g_cq"].reshape(2, 3, 128))
    d["w_uq"] = inp["w_uq"]
    wq = inp["w_uq"].reshape(2, 384, 6, 96)
    d["w_uq_sw"] = np.ascontiguousarray(np.concatenate([wq[..., 0:64], wq[..., 80:96], wq[..., 64:80]], -1).reshape(2, 384, 576))
    d["g_ckv"] = np.ascontiguousarray(inp["g_ckv"].reshape(2, 2, 128))
    d["w_ukv"] = inp["w_ukv"]
    d["cmp_posT"] = np.ascontiguousarray(np.stack([inp["cmp_k_pos"].transpose(0, 2, 1), inp["cmp_v_pos"].transpose(0, 2, 1)], 1))
    d["cmp_w1"] = np.ascontiguousarray(np.stack([inp["cmp_k_w1"], inp["cmp_v_w1"]], 1))
    d["cmp_w2"] = np.ascontiguousarray(np.stack([inp["cmp_k_w2"], inp["cmp_v_w2"]], 1))
    d["w_out"] = inp["w_out"]
    d["ln_gb"] = np.ascontiguousarray(np.stack([inp["ln1_g"], inp["ln1_b"], inp["ln2_g"], inp["ln2_b"]], 1))
    for k in ("ffn_w1", "ffn_w3", "ffn_w2", "router_w", "moe_w1", "moe_w3", "moe_w2"):
        d[k] = inp[k]
    d.update(host_consts())
    return {k: np.ascontiguousarray(v) for k, v in d.items()}


def kernel(**inputs):
    inp = {k: np.asarray(v) for k, v in inputs.items()}
    ncores = 8
    nseq = inp["x"].shape[0] // ncores
    nc, P, es, L = build(nseq=nseq)
    L["run_all"]()
    finish(nc, P, es, ["out0", "out1", "out2", "out3"])
    wd = prep_weights(inp)
    in_maps = []
    for i in range(ncores):
        m = dict(wd)
        m["x"] = np.ascontiguousarray(inp["x"][i * nseq:(i + 1) * nseq])
        in_maps.append(m)
    res = run_bass_kernel_spmd(nc, in_maps, core_ids=list(range(ncores)))
    return np.concatenate([np.asarray(r["out"]) for r in res.results], axis=0).astype(np.float32)
```

```python
import numpy as np
import ml_dtypes
from contextlib import ExitStack
import concourse.bass as bass
import concourse.mybir as mybir
from concourse.bass_utils import run_bass_kernel_spmd

F32 = mybir.dt.float32
BF16 = mybir.dt.bfloat16
AF = mybir.ActivationFunctionType
ALU = mybir.AluOpType

S = 2048
D = 1024
NT = 16
NSPAN = 4
DEPTH = 2
ALPHA = (2 * DEPTH) ** 0.25
EPS = 1e-5
NEG = -30720.0
C_CQ, C_CKV, C_KR = 0, 384, 640
C_FQ, C_FK, C_FV, C_FF = 672, 1056, 1440, 1824
C_NQ, C_KCMP, C_VCMP, C_KSLC, C_VSLC, C_KWIN, C_VWIN, C_GATE = 1830, 2086, 2150, 2214, 2278, 2342, 2406, 2470
IN_COLS = 2482
D_FF = 2816
N_EXP = 8
D_FFE = 1408

ENGS = ("pe", "act", "dve", "pool", "sp")


class Op:
    __slots__ = ("eng", "fn", "waits", "signal", "count", "dma_sem", "is_dma")

    def __init__(self, eng, fn):
        self.eng = eng
        self.fn = fn
        self.waits = []
        self.signal = False
        self.count = 0
        self.dma_sem = None
        self.is_dma = False


class Prog:
    def __init__(self, nc):
        self.nc = nc
        self.streams = {e: [] for e in ENGS}
        self.bufs = {}
        self.dma_cnt = {}
        self.fence_op = None

    def _deps(self, rd, wr):
        deps = []
        if self.fence_op is not None:
            for k in list(rd) + list(wr):
                if k not in self.bufs:
                    deps.append(self.fence_op)
                    break
        for k in rd:
            b = self.bufs.get(k)
            if b and b[0] is not None:
                deps.append(b[0])
        for k in wr:
            b = self.bufs.get(k)
            if b:
                if b[0] is not None:
                    deps.append(b[0])
                deps.extend(b[1])
        return deps

    def _register(self, op, rd, wr):
        for k in rd:
            b = self.bufs.setdefault(k, [None, []])
            b[1].append(op)
        for k in wr:
            self.bufs[k] = [op, []]

    def op(self, eng, fn, rd=(), wr=()):
        o = Op(eng, fn)
        for d in self._deps(rd, wr):
            if d.is_dma:
                o.waits.append(("dma", d.dma_sem, self.dma_cnt[d.dma_sem]))
            else:
                if d.eng == eng and eng == "pe":
                    continue
                d.signal = True
                o.waits.append(("op", d, 0))
        self._register(o, rd, wr)
        self.streams[eng].append(o)
        return o

    def fence(self, fn):
        o = self.op("dve", fn, rd=(), wr=list(self.bufs.keys()))
        self.bufs = {}
        self.fence_op = o
        return o

    def dma(self, eng, out, in_, sem, rd=(), wr=(), newgroup=True, **kw):
        o = self.op(eng, lambda e: e.dma_start(out=out, in_=in_, **kw), rd, wr)
        o.is_dma = True
        o.dma_sem = sem
        prev = self.dma_cnt.get(sem, 0)
        if newgroup and prev > 0:
            o.waits.append(("dma", sem, prev))
        self.dma_cnt[sem] = prev + 1
        return o

    def emit(self, es, final_waits):
        nc = self.nc
        sems = {e: es.enter_context(nc.semaphore("s_" + e)) for e in ENGS}
        dsems = {k: es.enter_context(nc.semaphore("d_" + k)) for k in self.dma_cnt}
        for e in ENGS:
            c = 0
            for o in self.streams[e]:
                if o.signal and not o.is_dma:
                    c += 1
                    o.count = c
        block = es.enter_context(nc.Block())
        streams = self.streams

        def run(e, eng):
            waited = {}
            for o in streams[e]:
                for w in o.waits:
                    if w[0] == "op":
                        key = ("e", w[1].eng)
                        val = w[1].count
                        sem = sems[w[1].eng]
                    else:
                        key = ("d", w[1])
                        val = 16 * w[2]
                        sem = dsems[w[1]]
                    if waited.get(key, 0) < val:
                        eng.wait_ge(sem, val)
                        waited[key] = val
                inst = o.fn(eng)
                if o.is_dma:
                    inst.then_inc(dsems[o.dma_sem], 16)
                elif o.signal:
                    inst.then_inc(sems[e], 1)
            if e == "sp":
                for semname in final_waits:
                    eng.wait_ge(dsems[semname], 16 * self.dma_cnt[semname])

        @block.tensor
        def _(t):
            run("pe", t)

        @block.scalar
        def _(t):
            run("act", t)

        @block.vector
        def _(t):
            run("dve", t)

        @block.gpsimd
        def _(t):
            run("pool", t)

        @block.sync
        def _(t):
            run("sp", t)


def host_consts():
    bf = ml_dtypes.bfloat16
    c = {}
    p = np.arange(128)
    maskC = np.where(p[:, None] <= p[None, :], 0.0, NEG).astype(np.float32)
    maskA = np.where(p[:, None] > p[None, :], 0.0, NEG).astype(np.float32)
    c["c_masks"] = np.stack([maskC, maskA], 1).astype(bf)
    t = np.arange(S)
    n = np.arange(128)
    mc = np.where(t[None, :] >= (16 * n[:, None] + 31), 0.0, NEG).astype(np.float32)
    c["c_maskcmp"] = mc.astype(bf)
    half = 16
    freqs = 10000.0 ** (-np.arange(half, dtype=np.float32) / half)
    ang = t.astype(np.float32)[None, :] * freqs[:, None]
    cos = np.cos(ang).astype(np.float32)
    sin = np.sin(ang).astype(np.float32)
    rope = np.zeros((2, 128, S), np.float32)
    rope[0, 64:80] = cos
    rope[0, 80:96] = cos
    rope[1, 64:80] = -sin
    rope[1, 80:96] = sin
    c["c_rope"] = rope.astype(bf)
    slopes = [2.0 ** (-8.0 * (i + 1) / 4) for i in range(4)]
    thi = (t // 16) * 16
    tlo = t % 16
    aq = np.zeros((4, 4, S), np.float32)
    for h in range(4):
        aq[h, 0] = -8 * slopes[h] * thi
        aq[h, 1] = -8 * slopes[h] * tlo
        aq[h, 2] = 8 * slopes[h]
        aq[h, 3] = 8 * slopes[h]
    c["c_alibi_q"] = aq.astype(bf)
    ak = np.zeros((2, 4, S), np.float32)
    ak[0, 0] = 1
    ak[0, 1] = 1
    ak[0, 2] = thi
    ak[0, 3] = tlo
    pc = 16 * np.arange(S) + 31
    ak[1, 0] = 1
    ak[1, 1] = 1
    ak[1, 2] = (pc // 16) * 16
    ak[1, 3] = pc % 16
    c["c_alibi_k"] = ak.astype(bf)
    E = (t[None, :] // 64 == np.arange(32)[:, None]).astype(np.float32)
    c["c_onehot"] = E.astype(bf)
    keep = np.ones((128, 16, 32), np.float32)
    add = np.zeros((128, 16, 32), np.float32)
    for tt in range(16):
        for pp in range(128):
            tok = tt * 128 + pp
            cur = tok // 64
            for j in range(32):
                forced = (j == 0) or (j == cur) or (j == cur - 1)
                future = j * 64 > tok
                if forced:
                    keep[pp, tt, j] = 0
                    add[pp, tt, j] = 1.0e4
                elif future:
                    keep[pp, tt, j] = 0
                    add[pp, tt, j] = -1.0
    c["c_keepadd"] = np.stack([keep, add], 1).astype(np.float32)
    starts = np.arange(127) * 16
    sel_start = np.arange(32) * 64
    ov = ((starts[:, None] < sel_start[None, :] + 64) & (starts[:, None] + 32 > sel_start[None, :])).astype(np.float32)
    ova = np.zeros((128, 33), np.float32)
    ova[:127, :32] = ov
    ova[:127, 32] = 1.0
    c["c_ovl"] = ova.astype(bf)
    selm = np.zeros((44, 12, 128), np.float32)
    for r in range(12):
        selm[r, r, :] = 1
        selm[32 + r, r, :] = 1
    c["c_gsel"] = selm.astype(bf)
    c["c_ones"] = np.ones((3, S), np.float32).astype(bf)
    c["c_negones"] = (-np.ones((3, S), np.float32)).astype(bf)
    return c


class Ctx:
    pass


def build(nseq=2, dbg=None, nlayers=DEPTH):
    dbg = dbg or {}
    nc = bass.Bass("TRN2", target_bir_lowering=False)
    P = Prog(nc)
    es = ExitStack()

    def din(name, shape, dt=F32):
        return nc.dram_tensor(name, list(shape), dt, kind="ExternalInput").ap()

    x_d = din("x", [nseq, S, D])
    w_in = din("w_in", [DEPTH, D, IN_COLS])
    w_krsw = din("w_krsw", [DEPTH, D, 96])
    b_forget = din("b_forget", [DEPTH, 6, 1])
    g_cq = din("g_cq", [DEPTH, 3, 128])
    w_uq = din("w_uq", [DEPTH, 384, 576])
    w_uq_sw = din("w_uq_sw", [DEPTH, 384, 576])
    g_ckv = din("g_ckv", [DEPTH, 2, 128])
    w_ukv = din("w_ukv", [DEPTH, 256, 768])
    cmp_posT = din("cmp_posT", [DEPTH, 2, 64, 32])
    cmp_w1 = din("cmp_w1", [DEPTH, 2, 2048, 128])
    cmp_w2 = din("cmp_w2", [DEPTH, 2, 128, 64])
    w_out = din("w_out", [DEPTH, D, D])
    ln_gb = din("ln_gb", [DEPTH, 4, D])
    ffn_w1 = din("ffn_w1", [1, D, D_FF])
    ffn_w3 = din("ffn_w3", [1, D, D_FF])
    ffn_w2 = din("ffn_w2", [1, D_FF, D])
    router_w = din("router_w", [1, D, N_EXP])
    moe_w1 = din("moe_w1", [1, N_EXP, D, D_FFE])
    moe_w3 = din("moe_w3", [1, N_EXP, D, D_FFE])
    moe_w2 = din("moe_w2", [1, N_EXP, D_FFE, D])
    hc = host_consts()
    cd = {k: din(k, v.shape, BF16 if v.dtype == ml_dtypes.bfloat16 else F32) for k, v in hc.items()}
    out_d = nc.dram_tensor("out", [nseq, S, D], F32, kind="ExternalOutput").ap()
    dbg_d = {k: nc.dram_tensor(k, list(shp), dt, kind="ExternalOutput").ap() for k, (shp, dt) in dbg.items()}

    def sb(name, shape, dt):
        return es.enter_context(nc.sbuf_tensor(name, list(shape), dt))

    xres = sb("xres", [128, NT, D], F32)
    xT = sb("xT", [128, 8, S], BF16)
    mixT = sb("mixT", [128, 8, S], BF16)
    wpool_t = sb("wpool", [128, 8 * 1024], BF16)
    wpool = [wpool_t[:, i * 1024:(i + 1) * 1024] for i in range(8)]
    QT = sb("QT", [128, S], BF16)
    KT = sb("KT", [128, S], BF16)
    QTs = [QT, wpool_t[:, 4096:6144]]
    KTs = [KT, wpool_t[:, 6144:8192]]
    wmod = [4]
    VA = [sb("VA0", [128, NT, 128], BF16), sb("VA1", [128, NT, 128], BF16)]
    PT = [sb("PT%d" % i, [128, 512], BF16) for i in range(3)]
    ident = sb("ident", [128, 128], BF16)
    masks = sb("masks", [128, 2, 128], BF16)
    ones_bf = sb("ones_bf", [128, 128], BF16)
    smalls = sb("smalls", [128, 64], F32)
    rs = sb("rs", [128, 512], F32)
    xb_ld = sb("xb_ld", [128, 1024], BF16)
    ARENA_B = 39 * 1024
    arena = sb("arena", [128, ARENA_B // 4], F32)
    psb = [es.enter_context(nc.psum_tensor("ps%d" % i, [128, 512], F32)) for i in range(8)]

    class Carver:
        def __init__(self):
            self.off = 0

        def take(self, nbytes, dt, shape=None):
            n4 = (nbytes + 3) // 4
            n4 = (n4 + 7) // 8 * 8
            assert (self.off + n4) * 4 <= ARENA_B, (self.off * 4, nbytes)
            v = arena[:, self.off:self.off + n4]
            self.off += n4
            if dt == BF16:
                v = v.bitcast(BF16)[:, 0:nbytes // 2]
            else:
                v = v[:, 0:nbytes // 4]
            return v

    wp_i = [0]

    wgen = {}

    def wslot():
        i = wp_i[0] % wmod[0]
        wp_i[0] += 1
        wgen[i] = True
        return ("wp", i), wpool[i]

    dmasem_i = [0]

    def wload(dst_ap, src_ap, slotkey, **kw):
        sem = "w%d" % slotkey[1]
        ng = wgen.get(slotkey[1], True)
        wgen[slotkey[1]] = False
        P.dma("pool", dst_ap, src_ap, sem, wr=[slotkey], newgroup=ng, **kw)

    gb_i = [0]

    def gbank():
        i = 6 + gb_i[0] % 2
        gb_i[0] += 1
        return ("ps", i), psb[i]

    sbk_i = [0]

    def sbank():
        i = sbk_i[0] % 4
        sbk_i[0] += 1
        return ("ps", i), psb[i]

    ob_i = [0]

    def obank():
        i = 4 + ob_i[0] % 2
        ob_i[0] += 1
        return ("ps", i), psb[i]

    pt_i = [0]

    def ptile():
        i = pt_i[0] % 3
        pt_i[0] += 1
        return ("PT", i), PT[i]

    def mm(out, lhsT, rhs, start, stop, rd, wr):
        P.op("pe", lambda e: e.matmul(out, lhsT=lhsT, rhs=rhs, start=start, stop=stop, skip_group_check=True),
             rd=rd, wr=wr)

    def act(out, in_, func, rd, wr, **kw):
        P.op("act", lambda e: e.activation(out=out, in_=in_, func=func, **kw), rd=rd, wr=wr)

    def dve(name, rd, wr, *a, **kw):
        P.op("dve", lambda e: getattr(e, name)(*a, **kw), rd=rd, wr=wr)

    def span_keys(name, sp):
        return [(name, 4 * sp + i) for i in range(4)]

    def all_keys(name):
        return [(name, i) for i in range(NT)]

    def w_in_view(l, c0, n):
        return w_in[l][:, c0:c0 + n].rearrange("(kc p) n -> p kc n", p=128)

    def load_win(l, c0, n, src=None):
        key, slot = wslot()
        v = slot[:, 0:8 * n].rearrange("p (a b) -> p a b", a=8)
        wload(v, src if src is not None else w_in_view(l, c0, n), key)
        return key, v

    def proj_fm(wkey, wv, m, sp, bank_key, bank, col0=0):
        for kc in range(8):
            mm(bank[0:m, :], wv[:, kc, col0:col0 + m], xT[:, kc, sp * 512:(sp + 1) * 512], kc == 0, kc == 7,
               rd=[wkey] + span_keys("xT", sp), wr=[bank_key])

    P.op("pool", lambda e: e.memset(ident[:], 0.0), wr=["ident"])
    P.op("pool", lambda e: e.affine_select(out=ident[:], in_=ident[:], pattern=[[-1, 128]], compare_op=ALU.not_equal,
                                           fill=1.0, base=0, channel_multiplier=1), rd=["ident"], wr=["ident"])
    P.op("pool", lambda e: e.memset(ones_bf[:], 1.0), wr=["ones_bf"])
    P.op("pool", lambda e: e.memset(smalls[:, 0:1], EPS), wr=["smalls_c"])
    P.op("pool", lambda e: e.memset(smalls[:, 1:2], 1.0), wr=["smalls_c"])
    P.op("pool", lambda e: e.memset(smalls[:, 4:5], 1e-30), wr=["smalls_c"])
    P.op("pool", lambda e: e.memset(VA[0][:], 1.0), wr=[("VA", 0)])
    P.op("pool", lambda e: e.memset(VA[1][:], 1.0), wr=[("VA", 1)])
    P.dma("sp", masks[:], cd["c_masks"], "cst", wr=["masks"])
    maskC = masks[:, 0, :]
    maskA = masks[:, 1, :]

    def transpose_to_xT(tt, src_key, xb, dstT, dst_name):
        bk, bank = gbank()
        bv = bank[:].bitcast(BF16)
        for c in range(8):
            P.op("pe", lambda e, c=c: e.transpose(bv[:, c * 128:(c + 1) * 128], xb[:, c * 128:(c + 1) * 128], ident[:]),
                 rd=[src_key, "ident"], wr=[bk])
        act(dstT[:, :, tt * 128:(tt + 1) * 128], bv.rearrange("p (c t) -> p c t", c=8), AF.Copy,
            rd=[], wr=[bk, (dst_name, tt)])

    def make_group(qt_key, kt_key, ktile, va_key, va, r0, r1, scale, sp, mode, post, qtile=None):
        QTt = QT if qtile is None else qtile
        q0 = sp * 512
        kts = list(range(0, 4 * sp + 4)) if mode == "causal" else list(range(max(0, 4 * sp - 2), 4 * sp + 4))
        tasks = []
        for kt in kts:
            lo = max(q0, kt * 128)
            hi = q0 + 512
            if mode == "window":
                hi = min(hi, (kt + 3) * 128)
            mk = []
            if kt * 128 >= q0:
                mk.append((kt * 128 - q0, 128, ident[:], maskC, ["ident", "masks"]))
            if mode == "window" and q0 <= (kt + 2) * 128 < q0 + 512:
                mk.append(((kt + 2) * 128 - q0, 128, ident[:], maskA, ["ident", "masks"]))
            tasks.append(dict(lhsT=ktile[r0:r1, kt * 128:(kt + 1) * 128], rhs=QTt[r0:r1, lo:hi], c0=lo - q0, c1=hi - q0,
                              masks=mk, prow=128, va=va[:, kt, :], rdk=[kt_key, qt_key], vak=va_key, scale=scale))
        return dict(tasks=tasks, post=post)

    def run_groups(groups, look=3, fillers=()):
        flat = [(g, i) for g in groups for i in range(len(g["tasks"]))]
        staged = {}
        fillers = list(fillers)
        fpos = [0]
        ntask = max(1, len(flat) - 4)

        def fill(idx):
            want = min(len(fillers), ((idx + 1) * len(fillers) + ntask - 1) // ntask)
            while fpos[0] < want:
                fillers[fpos[0]]()
                fpos[0] += 1

        def qk(idx):
            g, i = flat[idx]
            t = g["tasks"][i]
            skey, sbk = sbank()
            pr = t["prow"]
            mm(sbk[0:pr, t["c0"]:t["c1"]], t["lhsT"], t["rhs"], True, False, rd=t["rdk"], wr=[skey])
            for (off, w, idap, mkap, rdm) in t["masks"]:
                mm(sbk[0:pr, off:off + w], idap, mkap, False, False, rd=rdm, wr=[skey])
            staged[idx] = (skey, sbk)

        gorder = {id(g): k for k, g in enumerate(groups)}
        pending = []

        def flush(pred):
            keep = []
            for item in pending:
                if pred(item):
                    item[2]()
                else:
                    keep.append(item)
            pending[:] = keep

        for j in range(min(look, len(flat))):
            qk(j)
        for idx in range(len(flat)):
            if idx + look < len(flat):
                qk(idx + look)
            g, i = flat[idx]
            t = g["tasks"][i]
            n = len(g["tasks"])
            go = gorder[id(g)]
            if i == 0:
                flush(lambda it: it[1] <= go - 2)
                g["okey"], g["ob"] = obank()
            skey, sbk = staged.pop(idx)
            pkey, pt = ptile()
            pr = t["prow"]
            c0, c1 = t["c0"], t["c1"]
            act(pt[0:pr, c0:c1], sbk[0:pr, c0:c1], AF.Exp, rd=[], wr=[skey, pkey], scale=t["scale"])
            mm(g["ob"][:, c0:c1], t["va"], pt[0:pr, c0:c1], i == 0, i == n - 1, rd=[t["vak"], pkey], wr=[g["okey"]])
            if i == n - 1:
                pending.append((idx + 2, go, (lambda g=g: g["post"](g["okey"], g["ob"]))))
            flush(lambda it: it[0] <= idx)
            fill(idx)
        flush(lambda it: True)
        fill(10 ** 9)

    def post_simple(okey, ob, par, c, sp):
        a, b = (0, 64) if par == 0 else (64, 0)
        act(rs[a:a + 64, :], ob[b:b + 64, :], AF.Ln, rd=[], wr=[okey, "rs"])
        act(rs[a:a + 64, :], rs[a:a + 64, :], AF.Exp, rd=[], wr=["rs"], scale=-1.0)
        dve("tensor_tensor", ["rs"], [okey] + span_keys(("mixT", c), sp),
            out=mixT[a:a + 64, c, sp * 512:(sp + 1) * 512], in0=ob[a:a + 64, :], in1=rs[a:a + 64, :], op=ALU.mult)

    def dbg_dump(name, src_ap, rd):
        if name in dbg_d:
            P.dma("sp", dbg_d[name], src_ap, "dbg", rd=rd)

    def mla_phase(l):
        cv = Carver()
        cqn = cv.take(3 * S * 2, BF16).rearrange("p (c t) -> p c t", c=3)
        ckvn = cv.take(2 * S * 2, BF16).rearrange("p (c t) -> p c t", c=2)
        tmpf = mixT[:, 6:8, :].rearrange("p a b -> p (a b)")[:, 0:3072].bitcast(F32).rearrange("p (c t) -> p c t", c=3)
        rb = cv.take(512 * 4, F32)
        rope = cv.take(2 * S * 2, BF16).rearrange("p (c t) -> p c t", c=2)
        krope = cv.take(S * 2, BF16)
        t1 = cv.take(512 * 4, F32)
        t2 = rb
        gvec = cv.take(8 * 4, F32)
        TK = [(("mixT", c), i) for c in (6, 7) for i in range(NT)]
        P.dma("sp", rope, cd["c_rope"].rearrange("c p t -> p c t"), "cst", wr=["rope"])
        P.dma("sp", gvec[:, 0:3], g_cq[l].rearrange("c p -> p c"), "cst", wr=["gvec"], allow_slow_non_contiguous=True)
        P.dma("sp", gvec[:, 3:5], g_ckv[l].rearrange("c p -> p c"), "cst", wr=["gvec"], allow_slow_non_contiguous=True)
        P.op("pool", lambda e: e.memset(VA[1][:, :, 0:64], 1.0), wr=[("VA", 1)])

        def latent(c0, nch, dstT, dname, gcol0, inv_n):
            wk = []
            for c in range(nch):
                wk.append(load_win(l, c0 + c * 128, 128))
            for sp in range(NSPAN):
                for c in range(nch):
                    bk, bank = gbank()
                    proj_fm(wk[c][0], wk[c][1], 128, sp, bk, bank)
                    act(tmpf[:, c, :], bank[:, :], AF.Copy, rd=[], wr=[bk] + TK)
                    dve("tensor_tensor", [], TK + [("PT", c)], out=PT[c][:], in0=tmpf[:, c, :], in1=tmpf[:, c, :],
                        op=ALU.mult)
                bk, bank = gbank()
                for c in range(nch):
                    mm(bank[:, :], ones_bf[:], PT[c][:], c == 0, c == nch - 1, rd=["ones_bf", ("PT", c)], wr=[bk])
                act(rb, bank[:, :], AF.Ln, rd=["smalls_c"], wr=[bk, "rb"], scale=inv_n, bias=smalls[:, 0:1])
                act(rb, rb, AF.Exp, rd=[], wr=["rb"], scale=-0.5)
                for c in range(nch):
                    dve("scalar_tensor_tensor", ["gvec"], TK + ["rb", (dname, c, sp)],
                        out=dstT[:, c, sp * 512:(sp + 1) * 512], in0=tmpf[:, c, :], scalar=gvec[:, gcol0 + c:gcol0 + c + 1],
                        in1=rb, op0=ALU.mult, op1=ALU.mult)

        latent(C_CQ, 3, cqn, "cqn", 0, 1.0 / 384)
        latent(C_CKV, 2, ckvn, "ckvn", 3, 1.0 / 256)
        ka = load_win(l, 576, 96)
        kb = load_win(l, 0, 96, src=w_krsw[l].rearrange("(kc p) n -> p kc n", p=128))
        for sp in range(NSPAN):
            bkA, bA = gbank()
            proj_fm(ka[0], ka[1], 96, sp, bkA, bA)
            bkB, bB = gbank()
            proj_fm(kb[0], kb[1], 96, sp, bkB, bB)
            sl = slice(sp * 512, (sp + 1) * 512)
            dve("tensor_tensor", ["rope"], [bkA, "rs"], out=t1[64:96, :], in0=bA[64:96, :], in1=rope[64:96, 0, sl], op=ALU.mult)
            dve("tensor_tensor", ["rope"], [bkB, "rb"], out=t2[64:96, :], in0=bB[64:96, :], in1=rope[64:96, 1, sl], op=ALU.mult)
            dve("tensor_tensor", ["rs", "rb"], [("krope", sp)], out=krope[64:96, sl], in0=t1[64:96, :], in1=t2[64:96, :], op=ALU.add)
        cqn_keys = [("cqn", c, sp) for c in range(3) for sp in range(NSPAN)]
        ckvn_keys = [("ckvn", c, sp) for c in range(2) for sp in range(NSPAN)]
        def prep(h):
            par = h % 2
            QTh, KTh = QTs[par], KTs[par]
            qk_, kk_ = ("QT", par), ("KT", par)
            units = []
            st = {}

            def u_load():
                k1, s1 = wslot()
                st["k1"] = k1
                st["wq"] = s1[:, 0:288].rearrange("p (a b) -> p a b", a=3)
                st["wqs"] = s1[:, 288:576].rearrange("p (a b) -> p a b", a=3)
                st["wkv"] = s1[:, 576:832].rearrange("p (a b) -> p a b", a=2)
                wload(st["wq"], w_uq[l][:, h * 96:(h + 1) * 96].rearrange("(kc p) n -> p kc n", p=128), k1)
                wload(st["wqs"], w_uq_sw[l][:, h * 96:(h + 1) * 96].rearrange("(kc p) n -> p kc n", p=128), k1)
                wload(st["wkv"], w_ukv[l][:, h * 128:(h + 1) * 128].rearrange("(kc p) n -> p kc n", p=128), k1)
                dve("tensor_copy", [("krope", sp) for sp in range(NSPAN)], [kk_], out=KTh[64:96, :], in_=krope[64:96, :])
            units.append(u_load)

            def u_q(sp):
                k1, wq, wqs = st["k1"], st["wq"], st["wqs"]
                sl = slice(sp * 512, (sp + 1) * 512)
                bkA, bA = gbank()
                for c in range(3):
                    mm(bA[0:96, :], wq[:, c, :], cqn[:, c, sl], c == 0, c == 2, rd=[k1] + cqn_keys, wr=[bkA])
                dve("tensor_copy", [], [bkA, qk_], out=QTh[0:64, sl], in_=bA[0:64, :])
                dve("tensor_tensor", ["rope"], [bkA, "t1m"], out=t1[64:96, :], in0=bA[64:96, :], in1=rope[64:96, 0, sl], op=ALU.mult)
                bkB, bB = gbank()
                for c in range(3):
                    mm(bB[0:96, :], wqs[:, c, :], cqn[:, c, sl], c == 0, c == 2, rd=[k1] + cqn_keys, wr=[bkB])
                dve("tensor_tensor", ["rope"], [bkB, "rb"], out=t2[64:96, :], in0=bB[64:96, :], in1=rope[64:96, 1, sl], op=ALU.mult)
                dve("tensor_tensor", ["t1m", "rb"], [qk_], out=QTh[64:96, sl], in0=t1[64:96, :], in1=t2[64:96, :], op=ALU.add)

            def u_k(sp):
                k1, wkv = st["k1"], st["wkv"]
                sl = slice(sp * 512, (sp + 1) * 512)
                bkK, bK = gbank()
                for c in range(2):
                    mm(bK[0:64, :], wkv[:, c, 0:64], ckvn[:, c, sl], c == 0, c == 1, rd=[k1] + ckvn_keys, wr=[bkK])
                dve("tensor_copy", [], [bkK, kk_], out=KTh[0:64, sl], in_=bK[0:64, :])

            def u_v(g):
                k1, wkv = st["k1"], st["wkv"]
                voff = 0 if par == 0 else 64
                bkV, bV = gbank()
                for j in range(4):
                    tt = 4 * g + j
                    for c in range(2):
                        mm(bV[:, j * 64:(j + 1) * 64], ckvn[:, c, tt * 128:(tt + 1) * 128], wkv[:, c, 64:128],
                           (j == 0 and c == 0), (j == 3 and c == 1), rd=[k1] + ckvn_keys, wr=[bkV])
                dve("tensor_copy", [], [bkV, ("VA", par)], out=VA[par][:, 4 * g:4 * g + 4, voff:voff + 64],
                    in_=bV[:, 0:256].rearrange("p (j d) -> p j d", j=4))
            for sp in range(NSPAN):
                units.append(lambda sp=sp: u_q(sp))
                units.append(lambda sp=sp: u_k(sp))
            for g in range(4):
                units.append(lambda g=g: u_v(g))
            return units

        for u in prep(0):
            u()
        for h in range(6):
            par = h % 2
            cch = h // 2
            nxt = prep(h + 1) if h + 1 < 6 else []
            run_groups([make_group(("QT", par), ("KT", par), KTs[par], ("VA", par), VA[par], 0, 96, 96 ** -0.5, sp, "causal",
                                   (lambda okey, ob, par=par, cch=cch, sp=sp: post_simple(okey, ob, par, cch, sp)),
                                   qtile=QTs[par])
                        for sp in range(NSPAN)], fillers=nxt)

    def fox_phase(l):
        cv = Carver()
        big = cv.take(2 * S * 4, F32)
        nl = big[:, 0:S]
        a8 = big[:, S:2 * S]
        rr = nl
        Vall = big.bitcast(BF16).rearrange("p (t b d) -> p t b d", t=NT, b=8)
        CH = cv.take(3 * S * 2, BF16).rearrange("p (c t) -> p c t", c=3)
        bvec = cv.take(8 * 4, F32)
        P.dma("sp", bvec[0:6, 0:1], b_forget[l], "cst", wr=["bvec"])
        dve("tensor_scalar", ["bvec"], ["bvec"], out=bvec[0:6, 1:2], in0=bvec[0:6, 0:1], scalar1=-1.0, scalar2=None, op0=ALU.mult)
        wf = load_win(l, C_FF, 6)
        for sp in range(NSPAN):
            sl = slice(sp * 512, (sp + 1) * 512)
            bk, bank = gbank()
            proj_fm(wf[0], wf[1], 6, sp, bk, bank)
            act(nl[0:6, sl], bank[0:6, :], AF.Exp, rd=["bvec"], wr=[bk, "nl"], scale=-1.0, bias=bvec[0:6, 1:2])
            act(nl[0:6, sl], nl[0:6, sl], AF.Ln, rd=["smalls_c"], wr=["nl"], bias=smalls[0:6, 1:2])
        dve("tensor_tensor_scan", ["nl", "smalls_c"], ["a8"], out=a8[0:6, :], data0=smalls[0:6, 1:2].to_broadcast([6, S]), data1=nl[0:6, :], initial=0.0,
            op0=ALU.mult, op1=ALU.add)
        dve("tensor_scalar", ["a8"], ["a8"], out=a8[0:6, :], in0=a8[0:6, :], scalar1=8.0, scalar2=None, op0=ALU.mult)
        dve("tensor_copy", ["a8"], ["CH"], out=CH[0:6, 0, :], in_=a8[0:6, :])
        dve("tensor_tensor", ["a8", "CH"], ["nl"], out=rr[0:6, :], in0=a8[0:6, :], in1=CH[0:6, 0, :], op=ALU.subtract)
        dve("tensor_copy", ["nl"], ["CH"], out=CH[0:6, 1, :], in_=rr[0:6, :])
        dve("tensor_tensor", ["CH"], ["nl"], out=rr[0:6, :], in0=rr[0:6, :], in1=CH[0:6, 1, :], op=ALU.subtract)
        dve("tensor_copy", ["nl"], ["CH"], out=CH[0:6, 2, :], in_=rr[0:6, :])
        P.op("pool", lambda e: e.memset(Vall[:, :, 0, :], 1.0), wr=["nl", "a8", "Vall"])
        P.op("pool", lambda e: e.memset(Vall[:, :, 7, :], 1.0), wr=["Vall"])
        wp_i[0] = (wp_i[0] + 3) // 4 * 4
        vkeys = []
        for _ in range(3):
            k_, _s = wslot()
            vkeys.append(k_)
        wv_all = wpool_t[:, 0:3072].rearrange("p (a b) -> p a b", a=8)
        for i_, k_ in enumerate(vkeys):
            pass
        P.dma("pool", wv_all, w_in_view(l, C_FV, 384), "w0", wr=vkeys)
        for tt in range(NT):
            bkV, bV = gbank()
            for kc in range(8):
                mm(bV[:, 0:384], xT[:, kc, tt * 128:(tt + 1) * 128], wv_all[:, kc, :], kc == 0, kc == 7,
                   rd=vkeys + [("xT", tt)], wr=[bkV])
            act(Vall[:, tt, 1:7, :], bV[:, 0:384].rearrange("p (h d) -> p h d", h=6), AF.Copy, rd=[], wr=[bkV, "Vall"])

        def prep(h):
            par = h % 2
            QTh, KTh = QTs[par], KTs[par]
            qk_, kk_ = ("QT", par), ("KT", par)
            st = {}
            units = []

            def u_load():
                key, slot = wslot()
                v = slot[:, :].rearrange("p (a b) -> p a b", a=8)
                wload(v[:, :, 0:64], w_in_view(l, C_FQ + 64 * h, 64), key)
                wload(v[:, :, 64:128], w_in_view(l, C_FK + 64 * h, 64), key)
                st["w"] = (key, v)
                for r in range(3):
                    P.dma("sp", QTh[64 + r:65 + r, :], CH[h:h + 1, r, :], "aug", rd=["CH"], wr=[qk_], newgroup=(r == 0))
                    P.dma("sp", KTh[67 + r:68 + r, :], CH[h:h + 1, r, :], "aug", rd=["CH"], wr=[kk_], newgroup=False)
                P.dma("sp", QTh[67:70, :], cd["c_ones"], "aug", wr=[qk_], newgroup=False)
                P.dma("sp", KTh[64:67, :], cd["c_negones"], "aug", wr=[kk_], newgroup=False)
            units.append(u_load)

            def u_qk(sp):
                w = st["w"]
                sl = slice(sp * 512, (sp + 1) * 512)
                bk, bank = gbank()
                proj_fm(w[0], w[1], 128, sp, bk, bank)
                dve("tensor_copy", [], [bk, qk_], out=QTh[0:64, sl], in_=bank[0:64, :])
                dve("tensor_copy", [], [bk, kk_], out=KTh[0:64, sl], in_=bank[64:128, :])
            def u_v():
                voff = 0 if par == 0 else 64
                P.op("pool", lambda e: e.tensor_copy(out=VA[par][:, :, voff:voff + 64], in_=Vall[:, :, h + 1, :]),
                     rd=["Vall"], wr=[("VA", par)])
            for sp in range(NSPAN):
                units.append(lambda sp=sp: u_qk(sp))
            units.append(u_v)
            return units

        for u in prep(0):
            u()
        for h in range(6):
            par = h % 2
            cch = 3 + h // 2
            nxt = prep(h + 1) if h + 1 < 6 else []
            run_groups([make_group(("QT", par), ("KT", par), KTs[par], ("VA", par), VA[par], 0, 70, 0.125, sp, "causal",
                                   (lambda okey, ob, par=par, cch=cch, sp=sp: post_simple(okey, ob, par, cch, sp)),
                                   qtile=QTs[par])
                        for sp in range(NSPAN)], fillers=nxt)

    def nsa_phase(l):
        cv = Carver()
        SELB = cv.take(S * 2, BF16)
        KW = cv.take(S * 2, BF16)
        KC = cv.take(128 * 2, BF16)
        VC = cv.take(128 * 2, BF16)
        GT = cv.take(S * 2, BF16)
        maskcmp = cv.take(S * 2, BF16)
        kvT = cv.take(S * 2, BF16)
        imp = cv.take(NT * 32 * 4, F32).rearrange("p (a b) -> p a b", a=NT)
        impm = cv.take(NT * 32 * 4, F32).rearrange("p (a b) -> p a b", a=NT)
        keepadd = cv.take(2 * NT * 32 * 4, F32).rearrange("p (k a b) -> p k a b", k=2, a=NT)
        h1 = cv.take(2 * 128 * 2, BF16).rearrange("p (a b) -> p a b", a=2)
        gsel = cv.take(12 * 128 * 2, BF16).rearrange("p (a b) -> p a b", a=12)
        ovl = cv.take(64 * 2, BF16)
        pe2 = cv.take(32 * 2, BF16)
        acc = cv.take(512 * 4, F32)
        tmp = cv.take(512 * 4, F32)
        selbf = kvT[:, 0:1536].rearrange("p (a b) -> p a b", a=NT)
        fac = cv.take(512 * 4, F32)
        top8 = cv.take(NT * 8 * 4, F32).rearrange("p (a b) -> p a b", a=NT)
        r4 = cv.take(8 * 4, F32)
        P.dma("sp", KT[64:96, :], cd["c_onehot"], "cst", wr=[("KT", 0)])
        P.op("pool", lambda e: e.memset(KW[64:96, :], 0.0), wr=["KW"])
        P.op("pool", lambda e: e.memset(KC[64:96, :], 0.0), wr=["KC"])
        P.dma("sp", KT[96:100, :], cd["c_alibi_k"][0], "cst", wr=[("KT", 0)])
        P.dma("sp", KW[96:100, :], cd["c_alibi_k"][0], "cst", wr=["KW"])
        P.dma("sp", KC[96:100, :], cd["c_alibi_k"][1][:, 0:128], "cst", wr=["KC"])
        P.dma("sp", maskcmp, cd["c_maskcmp"], "cst", wr=["maskcmp"])
        P.dma("sp", keepadd, cd["c_keepadd"], "cst", wr=["keepadd"])
        P.dma("sp", gsel[0:44, :, :], cd["c_gsel"], "cst", wr=["gsel"])
        P.dma("sp", ovl[:, 0:33], cd["c_ovl"], "cst", wr=["ovl"])
        P.op("pool", lambda e: e.memset(VA[1][:, :, 64:128], 1.0), wr=[("VA", 1)])
        P.op("pool", lambda e: e.memset(VC[:, 64:128], 1.0), wr=["VC"])
        P.dma("pool", pe2[0:64, :], cmp_posT[l][0], "cst2", wr=["pe2"])
        P.dma("pool", pe2[64:128, :], cmp_posT[l][1], "cst2", wr=["pe2"])

        wkv = load_win(l, C_KCMP, 128)
        kkey, kslot = wslot()
        wkk = kslot[:, :].rearrange("p (a b) -> p a b", a=8)
        wload(wkk[:, :, 0:64], w_in_view(l, C_KSLC, 64), kkey)
        wload(wkk[:, :, 64:128], w_in_view(l, C_KWIN, 64), kkey)
        wg = load_win(l, C_GATE, 12)
        for sp in range(NSPAN):
            sl = slice(sp * 512, (sp + 1) * 512)
            bk, bank = gbank()
            proj_fm(wkv[0], wkv[1], 128, sp, bk, bank)
            act(kvT[:, sl], bank[:, :], AF.Copy, rd=[], wr=[bk, "kvT"])
            bk, bank = gbank()
            proj_fm(kkey, wkk, 128, sp, bk, bank)
            act(KT[0:64, sl], bank[0:64, :], AF.Copy, rd=[], wr=[bk, ("KT", 0)])
            dve("tensor_copy", [], [bk, "KW"], out=KW[0:64, sl], in_=bank[64:128, :])
            bk, bank = gbank()
            proj_fm(wg[0], wg[1], 12, sp, bk, bank)
            act(tmp[0:12, :], bank[0:12, :], AF.Sigmoid, rd=[], wr=[bk, "tmp"])
            dve("tensor_copy", ["tmp"], ["GT"], out=GT[0:12, sl], in_=tmp[0:12, :])
            dve("tensor_tensor", ["tmp"], ["GT"], out=GT[32:44, sl], in0=tmp[0:12, :], in1=GT[0:12, sl], op=ALU.subtract)
        vkey, vslot = wslot()
        wvv = vslot[:, :].rearrange("p (a b) -> p a b", a=8)
        wload(wvv[:, :, 0:64], w_in_view(l, C_VSLC, 64), vkey)
        wload(wvv[:, :, 64:128], w_in_view(l, C_VWIN, 64), vkey)
        for g in range(4):
            bkV, bV = gbank()
            for j in range(4):
                tt = 4 * g + j
                for kc in range(8):
                    mm(bV[:, j * 128:(j + 1) * 128], xT[:, kc, tt * 128:(tt + 1) * 128], wvv[:, kc, :],
                       (j == 0 and kc == 0), (j == 3 and kc == 7), rd=[vkey, ("xT", tt)], wr=[bkV])
            bv4 = bV[:, :].rearrange("p (j d) -> p j d", j=4)
            act(VA[0][:, 4 * g:4 * g + 4, 0:64], bv4[:, :, 0:64], AF.Copy, rd=[], wr=[bkV, ("VA", 0)])
            act(VA[1][:, 4 * g:4 * g + 4, 0:64], bv4[:, :, 64:128], AF.Copy, rd=[], wr=[bkV, ("VA", 1)])
        w1s = []
        for i in range(4):
            key, slot = wslot()
            v = slot[:, :].rearrange("p (a b) -> p a b", a=8)
            for kv in range(2):
                src = cmp_w1[l][kv].rearrange("(l d) j -> d l j", d=64)[:, 8 * i:8 * i + 8, :]
                wload(v[64 * kv:64 * kv + 64, :, :], src, key)
            w1s.append((key, v))
        key2 = "w2cmp"
        w2t = cv.take(128 * 2, BF16)
        w2k = w2t[:, 0:64]
        w2v = w2t[:, 64:128]
        P.dma("pool", w2k, cmp_w2[l][0], "cst2", wr=[key2])
        P.dma("pool", w2v, cmp_w2[l][1], "cst2", wr=[key2], newgroup=False)
        for kv in range(2):
            pr = slice(64 * kv, 64 * kv + 64)
            bk, bank = gbank()
            for li in range(32):
                wkey, wvw_ = w1s[li // 8]
                mm(bank[:, 0:127], wvw_[pr, li % 8, :], kvT[pr, li:li + 16 * 126 + 1:16], li == 0 and True, False,
                   rd=[wkey, "kvT"], wr=[bk])
                mm(bank[:, 127:128], wvw_[pr, li % 8, :], pe2[pr, li:li + 1], False, li == 31, rd=[wkey, "pe2"], wr=[bk])
            act(smalls[:, 2 + kv:3 + kv], bank[:, 127:128], AF.Copy, rd=[], wr=[bk, ("sm", 2 + kv)])
            act(h1[:, kv, 0:127], bank[:, 0:127], AF.Silu, rd=[("sm", 2 + kv)], wr=[bk, ("h1", kv)], bias=smalls[:, 2 + kv:3 + kv])
        bk, bank = gbank()
        mm(bank[0:64, 0:127], w2k, h1[:, 0, 0:127], True, True, rd=[key2, ("h1", 0)], wr=[bk])
        act(KC[0:64, 0:127], bank[0:64, 0:127], AF.Copy, rd=[], wr=[bk, "KC"])
        bk, bank = gbank()
        mm(bank[0:127, 0:64], h1[:, 1, 0:127], w2v, True, True, rd=[key2, ("h1", 1)], wr=[bk])
        act(VC[0:127, 0:64], bank[0:127, 0:64], AF.Copy, rd=[], wr=[bk, "VC"])

        def load_q_units(h, with_sel):
            qs = h % 2
            QTh = QTs[qs]
            st = {}
            units = []

            def u0():
                st["wq"] = load_win(l, C_NQ + 64 * h, 64)
                P.dma("sp", QTh[96:100, :], cd["c_alibi_q"][h], "aug", wr=[("QT", qs)])
                if with_sel:
                    dve("tensor_copy", ["SELB"], [("QT", qs)], out=QTh[64:96, :], in_=SELB[64:96, :])
            units.append(u0)

            def u1(sp):
                wq = st["wq"]
                sl = slice(sp * 512, (sp + 1) * 512)
                bk, bank = gbank()
                proj_fm(wq[0], wq[1], 64, sp, bk, bank)
                dve("tensor_copy", [], [bk, ("QT", qs)], out=QTh[0:64, sl], in_=bank[0:64, :])
            for sp in range(NSPAN):
                units.append(lambda sp=sp: u1(sp))
            return units

        def load_q(h, with_sel=False):
            for u in load_q_units(h, with_sel):
                u()

        def cmp_scores(sp, qs):
            sl = slice(sp * 512, (sp + 1) * 512)
            skey, sbk = sbank()
            mm(sbk[0:127, :], KC[0:100, 0:127], QTs[qs][0:100, sl], True, False, rd=["KC", ("QT", qs)], wr=[skey])
            mm(sbk[0:127, :], ident[0:127, 0:127], maskcmp[0:127, sl], False, True, rd=["ident", "maskcmp"], wr=[skey])
            pkey, pt = ptile()
            act(pt[0:127, :], sbk[0:127, :], AF.Exp, rd=[], wr=[skey, pkey], scale=0.125)
            return pkey, pt

        load_q(0)
        p1 = [(h, sp) for h in range(4) for sp in range(NSPAN)]
        staged1 = None
        for i1, (h, sp) in enumerate(p1):
            if True:
                if sp == 0 and h + 1 < 4:
                    load_q(h + 1)
                pkey, pt = staged1 if staged1 is not None else cmp_scores(sp, h % 2)
                staged1 = cmp_scores(p1[i1 + 1][1], p1[i1 + 1][0] % 2) if i1 + 1 < len(p1) else None
                bk, bank = gbank()
                for j in range(4):
                    mm(bank[:, j * 33:(j + 1) * 33], pt[0:127, j * 128:(j + 1) * 128], ovl[0:127, 0:33], j == 0, j == 3,
                       rd=[pkey, "ovl"], wr=[bk])
                bv = bank[:, 0:132].rearrange("p (j c) -> p j c", j=4)
                dve("tensor_scalar_max", [], [bk, "r4"], out=r4[:, 0:4], in0=bv[:, :, 32], scalar1=1e-30)
                dve("reciprocal", [], ["r4"], out=r4[:, 0:4], in_=r4[:, 0:4])
                r4b = r4[:, 0:4].unsqueeze(2).to_broadcast([128, 4, 32])
                if h == 0:
                    dve("tensor_tensor", ["r4"], [bk, ("imp", sp)], out=imp[:, 4 * sp:4 * sp + 4, :], in0=bv[:, :, 0:32], in1=r4b,
                        op=ALU.mult)
                else:
                    tv = tmp[:, 0:128].rearrange("p (a b) -> p a b", a=4)
                    dve("tensor_tensor", ["r4"], [bk, "tmp"], out=tv, in0=bv[:, :, 0:32], in1=r4b, op=ALU.mult)
                    dve("tensor_tensor", ["tmp"], [("imp", sp)], out=imp[:, 4 * sp:4 * sp + 4, :], in0=imp[:, 4 * sp:4 * sp + 4, :],
                        in1=tv, op=ALU.add)
        impk = [("imp", sp) for sp in range(NSPAN)]
        dve("tensor_tensor", impk + ["keepadd"], ["impm"], out=impm[:, :, :], in0=imp[:, :, :], in1=keepadd[:, 0, :, :], op=ALU.mult)
        dve("tensor_tensor", ["keepadd"], ["impm"], out=impm[:, :, :], in0=impm[:, :, :], in1=keepadd[:, 1, :, :], op=ALU.add)
        for tt in range(NT):
            dve("max", ["impm"], [("top8", tt)], out=top8[:, tt, :], in_=impm[:, tt, :])
        for tt in range(NT):
            dve("tensor_scalar", [("top8", tt)], ["impm"], out=impm[:, tt, :], in0=impm[:, tt, :], scalar1=top8[:, tt, 7:8],
                scalar2=None, op0=ALU.is_ge)
        dve("tensor_scalar", [], ["impm", "kvT", "selbf"], out=selbf[:, :, 64:96], in0=impm[:, :, :], scalar1=-1.0, scalar2=-NEG,
            op0=ALU.add, op1=ALU.mult)
        for half in range(2):
            bk, bank = gbank()
            bv = bank[:].bitcast(BF16)
            for j in range(8):
                tt = 8 * half + j
                P.op("pe", lambda e, j=j, tt=tt, bv=bv: e.transpose(bv[0:96, j * 128:(j + 1) * 128], selbf[:, tt, :], ident[:]),
                     rd=["selbf", "ident"], wr=[bk])
            act(SELB[64:96, half * 1024:(half + 1) * 1024], bv[64:96, :], AF.Copy, rd=[], wr=[bk, "SELB"])
        dbg_dump("d_selb", SELB[64:96, :], ["SELB"])
        dve("memset", [], ["kvT", "fac"], fac[0:64, 0:1], 0.0)
        def cmp_group(sp, post, qs):
            sl = slice(sp * 512, (sp + 1) * 512)
            t = dict(lhsT=KC[0:100, 0:127], rhs=QTs[qs][0:100, sl], c0=0, c1=512,
                     masks=[(0, 512, ident[0:127, 0:127], maskcmp[0:127, sl], ["ident", "maskcmp"])], prow=127,
                     va=VC[0:127, :], rdk=["KC", ("QT", qs)], vak="VC", scale=0.125)
            return dict(tasks=[t], post=post)

        load_q(0, True)
        for h in range(4):
            par = h % 2
            cch = 6 + h // 2
            qs = h % 2

            def branch_post(okey, ob, b, sp, h=h, par=par, cch=cch):
                sl = slice(sp * 512, (sp + 1) * 512)
                gk, gb = gbank()
                mm(gb[:, :], gsel[0:44, 3 * h + b, :], GT[0:44, sl], True, True, rd=["gsel", "GT"], wr=[gk])
                if b == 0:
                    act(rs[0:64, :], ob[64:128, :], AF.Ln, rd=["smalls_c"], wr=[okey, "rs"], bias=smalls[0:64, 4:5])
                else:
                    act(rs[0:64, :], ob[64:128, :], AF.Ln, rd=[], wr=[okey, "rs"])
                act(rs[0:64, :], rs[0:64, :], AF.Exp, rd=[], wr=["rs"], scale=-1.0)
                dve("tensor_tensor", ["rs"], [gk, "fac"], out=fac[0:64, :], in0=gb[0:64, :], in1=rs[0:64, :], op=ALU.mult)
                if b == 0:
                    dve("tensor_tensor", ["fac"], [okey, "acc"], out=acc[0:64, :], in0=ob[0:64, :], in1=fac[0:64, :], op=ALU.mult)
                else:
                    dve("tensor_tensor", ["fac"], [okey, "tmp"], out=tmp[0:64, :], in0=ob[0:64, :], in1=fac[0:64, :], op=ALU.mult)
                    P.op("pool", lambda e: e.tensor_tensor(out=acc[0:64, :], in0=acc[0:64, :], in1=tmp[0:64, :], op=ALU.add),
                         rd=["tmp"], wr=["acc"])
                if b == 2:
                    a_ = 64 * par
                    dve("tensor_copy", ["acc"], span_keys(("mixT", cch), sp), out=mixT[a_:a_ + 64, cch, sl], in_=acc[0:64, :])

            groups = []
            for sp in range(NSPAN):
                groups.append(cmp_group(sp, (lambda okey, ob, sp=sp: branch_post(okey, ob, 0, sp)), qs))
                groups.append(make_group(("QT", qs), ("KT", 0), KT, ("VA", 0), VA[0], 0, 100, 0.125, sp, "causal",
                                         (lambda okey, ob, sp=sp: branch_post(okey, ob, 1, sp)), qtile=QTs[qs]))
                groups.append(make_group(("QT", qs), "KW", KW, ("VA", 1), VA[1], 0, 100, 0.125, sp, "window",
                                         (lambda okey, ob, sp=sp: branch_post(okey, ob, 2, sp)), qtile=QTs[qs]))
            run_groups(groups, fillers=(load_q_units(h + 1, True) if h + 1 < 4 else []))

    def post_phase(l, sq):
        cv = Carver()
        lnp = cv.take(2 * D * 4, F32).rearrange("p (a b) -> p a b", a=2)
        stats = cv.take(12 * 4, F32)
        mv = cv.take(8 * 4, F32)
        sc = cv.take(8 * 4, F32)
        tmps = [cv.take(512 * 4, F32), cv.take(512 * 4, F32)]
        xb2 = cv.take(D * 2, BF16)
        rwf = cv.take(64 * 4, F32).rearrange("p (a b) -> p a b", a=8)
        rwh = cv.take(2 * 64 * 2, BF16).rearrange("p (k a b) -> p k a b", k=2, a=8)
        lg = cv.take(128 * 4, F32).rearrange("p (a b) -> p a b", a=NT)
        eg = cv.take(128 * 4, F32).rearrange("p (a b) -> p a b", a=NT)
        mk = cv.take(128 * 4, F32).rearrange("p (a b) -> p a b", a=NT)
        gates = cv.take(128 * 4, F32).rearrange("p (a b) -> p a b", a=NT)
        m8 = cv.take(128 * 4, F32).rearrange("p (a b) -> p a b", a=NT)
        sm16 = cv.take(64 * 4, F32)
        moe = (l % 2 == 1)
        hT = mixT
        wmod[0] = 8

        def load_ln(which):
            P.dma("sp", lnp, ln_gb[l][2 * which:2 * which + 2, :].partition_broadcast(128), "cst", wr=["lnp"])

        xbs = [cv.take(D * 2, BF16), cv.take(D * 2, BF16)]
        xb2s = [xb2, cv.take(D * 2, BF16)]
        smallv = cv.take(64 * 4, F32)

        def ln_a(tt):
            pq = tt % 2
            xk = ("xres", tt)
            st = smallv[:, 12 * pq:12 * pq + 12]
            mv_ = smallv[:, 24 + 2 * pq:26 + 2 * pq]
            rst = smallv[:, 28 + pq:29 + pq]
            kst, kmv, krs = ("st", pq), ("mv", pq), ("rst", pq)
            dve("bn_stats", [], [xk, kst], out=st[:, 0:6], in_=xres[:, tt, 0:512])
            dve("bn_stats", [], [xk, kst], out=st[:, 6:12], in_=xres[:, tt, 512:1024])
            dve("bn_aggr", [], [kst, kmv], out=mv_, in_=st)
            act(rst, mv_[:, 1:2], AF.Sqrt, rd=["smalls_c"], wr=[kmv, krs], bias=smalls[:, 0:1])

        def ln_b(tt, final, make_lo):
            pq = tt % 2
            xk = ("xres", tt)
            xt_ = xres[:, tt, :]
            mv_ = smallv[:, 24 + 2 * pq:26 + 2 * pq]
            rst = smallv[:, 28 + pq:29 + pq]
            kst, kmv, krs = ("st", pq), ("mv", pq), ("rst", pq)
            dve("reciprocal", [], [krs], out=rst, in_=rst)
            dve("scalar_tensor_tensor", ["lnp", kmv], [xk], out=xt_, in0=xt_, scalar=mv_[:, 0:1], in1=lnp[:, 0, :],
                op0=ALU.subtract, op1=ALU.mult)
            dve("scalar_tensor_tensor", ["lnp", krs], [xk], out=xt_, in0=xt_, scalar=rst, in1=lnp[:, 1, :],
                op0=ALU.mult, op1=ALU.add)
            if final:
                P.dma("sp", out_d[sq, tt * 128:(tt + 1) * 128, :], xt_, "out%d" % (tt % 4), rd=[xk])
                return
            xb = xbs[pq]
            xbk = ("xb", pq)
            act(xb[:], xt_, AF.Copy, rd=[xk], wr=[xbk])
            if make_lo:
                x2 = xb2s[pq]
                x2k = ("xb2", pq)
                dve("tensor_tensor", [xk, xbk], [x2k], out=x2, in0=xt_, in1=xb[:], op=ALU.subtract)

        def ln_c(tt, final, make_lo):
            if final:
                return
            pq = tt % 2
            xb = xbs[pq]
            xbk = ("xb", pq)
            if make_lo:
                x2 = xb2s[pq]
                x2k = ("xb2", pq)
            transpose_to_xT(tt, xbk, xb, xT, "xT")
            if make_lo:
                bk, bank = gbank()
                bv = bank[:].bitcast(BF16)
                for c in range(8):
                    P.op("pe", lambda e, c=c, bv=bv, x2=x2: e.transpose(bv[:, c * 128:(c + 1) * 128], x2[:, c * 128:(c + 1) * 128], ident[:]),
                         rd=[x2k, "ident"], wr=[bk])
                act(mixT[:, :, tt * 128:(tt + 1) * 128], bv.rearrange("p (c t) -> p c t", c=8), AF.Copy,
                    rd=[], wr=[bk] + [(("mixT", c), tt) for c in range(8)])

        load_ln(0)
        wo = []
        for c in range(8):
            key, slot = wslot()
            wload(slot[:, :], w_out[l][c * 128:(c + 1) * 128, :], key)
            wo.append((key, slot))
        pb_i = [0]

        def pbank():
            i = pb_i[0] % 6
            pb_i[0] += 1
            return ("ps", i), psb[i]

        def outproj(tt):
            for half in range(2):
                bk, bank = pbank()
                for c in range(8):
                    mm(bank[:, :], mixT[:, c, tt * 128:(tt + 1) * 128], wo[c][1][:, half * 512:(half + 1) * 512], c == 0, c == 7,
                       rd=[wo[c][0], (("mixT", c), tt)], wr=[bk])
                xs_ = xres[:, tt, half * 512:(half + 1) * 512]
                dve("scalar_tensor_tensor", [], [bk, ("xres", tt)], out=xs_, in0=xs_, scalar=ALPHA, in1=bank[:, :],
                    op0=ALU.mult, op1=ALU.add)

        outproj(0)
        outproj(1)
        outproj(2)
        ln_a(0)
        for tt in range(NT):
            if tt + 3 < NT:
                outproj(tt + 3)
            if tt + 1 < NT:
                ln_a(tt + 1)
            ln_b(tt, False, moe)
            if tt >= 1:
                ln_c(tt - 1, False, moe)
        ln_c(NT - 1, False, moe)
        load_ln(1)

        ffn_groups = []
        wa_i = [0]
        wb_i = [0]

        def wslotA():
            i = wa_i[0] % 4
            wa_i[0] += 1
            wgen[i] = True
            return ("wp", i), wpool[i]

        def wslotB():
            i = 4 + wb_i[0] % 4
            wb_i[0] += 1
            wgen[i] = True
            return ("wp", i), wpool[i]

        def expert(w1d, w3d, w2d, nchunk, gate_e, first_scale):
            gl = [list(range(i, min(i + 4, nchunk))) for i in range(0, nchunk, 4)]
            for gi, grp in enumerate(gl):
                ffn_groups.append(dict(w1d=w1d, w3d=w3d, w2d=w2d, grp=grp, gate_e=gate_e, scale=(first_scale and gi == 0)))

        def run_ffn(tail, tail2):
            ng = len(ffn_groups)
            for g, G in enumerate(ffn_groups):
                grp = G["grp"]
                hb = 4 * (g % 2)
                w2s = []

                def load_w2():
                    for ci, c in enumerate(grp):
                        key, slot = wslotB()
                        wload(slot[:, :], G["w2d"][c * 128:(c + 1) * 128, :], key)
                        w2s.append((key, slot))
                for ci, c in enumerate(grp):
                    ws = []
                    for wd in (G["w1d"], G["w3d"]):
                        key, slot = wslotA()
                        v = slot[:, :].rearrange("p (a b) -> p a b", a=8)
                        wload(v, wd[:, c * 128:(c + 1) * 128].rearrange("(kc p) n -> p kc n", p=128), key)
                        ws.append((key, v))
                    if ci == min(1, len(grp) - 1):
                        load_w2()
                    (k1, v1), (k3, v3) = ws
                    for sp in range(NSPAN):
                        sl = slice(sp * 512, (sp + 1) * 512)
                        b1k, b1 = sbank()
                        proj_fm(k1, v1, 128, sp, b1k, b1)
                        b3k, b3 = gbank()
                        proj_fm(k3, v3, 128, sp, b3k, b3)
                        tsp = tmps[sp % 2]
                        tk = ("tmps", sp % 2)
                        act(tsp, b1[:, :], AF.Silu, rd=[], wr=[b1k, tk])
                        dve("tensor_tensor", [tk], [b3k] + span_keys(("mixT", hb + ci), sp), out=hT[:, hb + ci, sl], in0=b3[:, :],
                            in1=tsp, op=ALU.mult)
                gate_e = G["gate_e"]
                for tt in range(NT):
                    for half in range(2):
                        bk, bank = obank()
                        for ci in range(len(grp)):
                            mm(bank[:, :], hT[:, hb + ci, tt * 128:(tt + 1) * 128], w2s[ci][1][:, half * 512:(half + 1) * 512],
                               ci == 0, ci == len(grp) - 1, rd=[w2s[ci][0], (("mixT", hb + ci), tt)], wr=[bk])
                        xs_ = xres[:, tt, half * 512:(half + 1) * 512]
                        if gate_e is None:
                            if G["scale"]:
                                dve("scalar_tensor_tensor", [], [bk, ("xres", tt)], out=xs_, in0=xs_, scalar=ALPHA, in1=bank[:, :],
                                    op0=ALU.mult, op1=ALU.add)
                            else:
                                dve("tensor_tensor", [], [bk, ("xres", tt)], out=xs_, in0=xs_, in1=bank[:, :], op=ALU.add)
                        else:
                            dve("scalar_tensor_tensor", ["gates"], [bk, ("xres", tt)], out=xs_, in0=bank[:, :],
                                scalar=gates[:, tt, gate_e:gate_e + 1], in1=xs_, op0=ALU.mult, op1=ALU.add)
                    if g == ng - 1:
                        if tt >= 1:
                            ln_a(tt - 1)
                        if tt >= 2:
                            tail(tt - 2)
                        if tt >= 3:
                            tail2(tt - 3)
            ln_a(NT - 1)
            tail(NT - 2)
            tail2(NT - 3)
            tail(NT - 1)
            tail2(NT - 2)
            tail2(NT - 1)

        if not moe:
            j = l // 2
            expert(ffn_w1[j], ffn_w3[j], ffn_w2[j], D_FF // 128, None, True)
            run_ffn(lambda tt: ln_b(tt, l == nlayers - 1, False), lambda tt: ln_c(tt, l == nlayers - 1, False))
        else:
            j = l // 2
            P.dma("sp", rwf, router_w[j].rearrange("(kc p) e -> p kc e", p=128), "cst", wr=["rwf"])
            dve("tensor_copy", ["rwf"], ["rwh"], out=rwh[:, 0, :, :], in_=rwf)
            dve("tensor_tensor", ["rwf"], ["rwh"], out=rwh[:, 1, :, :], in0=rwf, in1=rwh[:, 0, :, :], op=ALU.subtract)
            bk, bank = gbank()
            n = 0
            for tt in range(NT):
                tsl = slice(tt * 128, (tt + 1) * 128)
                combos = [(xT, 0, ("xT", tt)), (mixT, 0, None), (xT, 1, ("xT", tt))]
                for ci_, (src, wi, key) in enumerate(combos):
                    for kc in range(8):
                        rdk = ["rwh"] + ([key] if key else [(("mixT", c), tt) for c in range(8)])
                        mm(bank[:, tt * 8:(tt + 1) * 8], src[:, kc, tsl], rwh[:, wi, kc, :], n == 0, False, rd=rdk, wr=[bk])
                        n += 1
            act(lg[:, :, :], bank[:, 0:128].rearrange("p (a b) -> p a b", a=NT), AF.Copy, rd=[], wr=[bk, "lg"])
            for tt in range(NT):
                dve("max", ["lg"], ["m8"], out=m8[:, tt, :], in_=lg[:, tt, :])
            dve("tensor_scalar", ["m8"], ["sm16"], out=sm16[:, 0:16], in0=m8[:, :, 0], scalar1=-1.0, scalar2=None, op0=ALU.mult)
            for tt in range(NT):
                act(eg[:, tt, :], lg[:, tt, :], AF.Exp, rd=["lg", "sm16"], wr=["eg"], bias=sm16[:, tt:tt + 1])
                dve("tensor_scalar", ["lg", "m8"], ["mk"], out=mk[:, tt, :], in0=lg[:, tt, :], scalar1=m8[:, tt, 1:2], scalar2=None,
                    op0=ALU.is_ge)
            dve("tensor_tensor", ["mk"], ["eg"], out=eg[:, :, :], in0=eg[:, :, :], in1=mk[:, :, :], op=ALU.mult)
            dve("tensor_reduce", ["eg"], ["sm16"], out=sm16[:, 16:32], in_=eg[:, :, :], axis=mybir.AxisListType.X, op=ALU.add)
            dve("reciprocal", [], ["sm16"], out=sm16[:, 16:32], in_=sm16[:, 16:32])
            dve("tensor_tensor", ["eg", "sm16"], ["gates"], out=gates[:, :, :], in0=eg[:, :, :],
                in1=sm16[:, 16:32].unsqueeze(2).to_broadcast([128, NT, 8]), op=ALU.mult)
            for tt in range(NT):
                dve("tensor_scalar", [], [("xres", tt)], out=xres[:, tt, :], in0=xres[:, tt, :], scalar1=ALPHA, scalar2=None,
                    op0=ALU.mult)
            for e_ in range(N_EXP):
                expert(moe_w1[j][e_], moe_w3[j][e_], moe_w2[j][e_], D_FFE // 128, e_, False)
            run_ffn(lambda tt: ln_b(tt, l == nlayers - 1, False), lambda tt: ln_c(tt, l == nlayers - 1, False))

    def run_all():
        for sq in range(nseq):
            load_x(sq)
            for l in range(nlayers):
                for ph in (mla_phase, fox_phase, nsa_phase):
                    P.fence(lambda e: e.memset(smalls[:, 60:61], 0.0))
                    ph(l)
                P.fence(lambda e: e.memset(smalls[:, 60:61], 0.0))
                post_phase(l, sq)
                wmod[0] = 4

    def load_x(sq):
        for tt in range(NT):
            P.dma("sp", xres[:, tt, :], x_d[sq, tt * 128:(tt + 1) * 128, :], "xin%d" % (tt % 4), wr=[("xres", tt)])
            act(xb_ld[:], xres[:, tt, :], AF.Copy, rd=[("xres", tt)], wr=["xb_ld"])
            transpose_to_xT(tt, "xb_ld", xb_ld, xT, "xT")

    stage = dbg.get("stage", (None, None))[0] if False else None
    PH = dict(mla=mla_phase, fox=fox_phase)
    return nc, P, es, locals()


def finish(nc, P, es, final_waits):
    P.emit(es, final_waits)
    es.close()
    return nc


def prep_weights(inp):
    d = {}
    w = inp["w_in"]
    d["w_in"] = w
    kr = w[:, :, 640:672]
    d["w_krsw"] = np.ascontiguousarray(np.concatenate([w[:, :, 576:640], kr[:, :, 16:32], kr[:, :, 0:16]], -1))
    d["b_forget"] = np.ascontiguousarray(inp["b_forget"].reshape(2, 6, 1))
    d["g_cq"] = np.ascontiguousarray(inp["g_cq"].reshape(2, 3, 128))
    d["w_uq"] = inp["w_uq"]
    wq = inp["w_uq"].reshape(2, 384, 6, 96)
    d["w_uq_sw"] = np.ascontiguousarray(np.concatenate([wq[..., 0:64], wq[..., 80:96], wq[..., 64:80]], -1).reshape(2, 384, 576))
    d["g_ckv"] = np.ascontiguousarray(inp["g_ckv"].reshape(2, 2, 128))
    d["w_ukv"] = inp["w_ukv"]
    d["cmp_posT"] = np.ascontiguousarray(np.stack([inp["cmp_k_pos"].transpose(0, 2, 1), inp["cmp_v_pos"].transpose(0, 2, 1)], 1))
    d["cmp_w1"] = np.ascontiguousarray(np.stack([inp["cmp_k_w1"], inp["cmp_v_w1"]], 1))
    d["cmp_w2"] = np.ascontiguousarray(np.stack([inp["cmp_k_w2"], inp["cmp_v_w2"]], 1))
    d["w_out"] = inp["w_out"]
    d["ln_gb"] = np.ascontiguousarray(np.stack([inp["ln1_g"], inp["ln1_b"], inp["ln2_g"], inp["ln2_b"]], 1))
    for k in ("ffn_w1", "ffn_w3", "ffn_w2", "router_w", "moe_w1", "moe_w3", "moe_w2"):
        d[k] = inp[k]
    d.update(host_consts())
    return {k: np.ascontiguousarray(v) for k, v in d.items()}


def kernel(**inputs):
    inp = {k: np.asarray(v) for k, v in inputs.items()}
    ncores = 8
    nseq = inp["x"].shape[0] // ncores
    nc, P, es, L = build(nseq=nseq)
    L["run_all"]()
    finish(nc, P, es, ["out0", "out1", "out2", "out3"])
    wd = prep_weights(inp)
    in_maps = []
    for i in range(ncores):
        m = dict(wd)
        m["x"] = np.ascontiguousarray(inp["x"][i * nseq:(i + 1) * nseq])
        in_maps.append(m)
    res = run_bass_kernel_spmd(nc, in_maps, core_ids=list(range(ncores)))
    return np.concatenate([np.asarray(r["out"]) for r in res.results], axis=0).astype(np.float32)
```
